# Optimizing a Trainium2 kernel written in Bass

```python
import jax, jax.numpy as jnp
from jax import lax
import numpy as np

D_MODEL = 1024
BATCH = 8
SEQ = 2048
DEPTH = 4

HEAD_DIM = D_MODEL // 16
CONV_HEADS = 6
POOL_GROUPS = 4
SGU_HEADS = 6
CONV_W = CONV_HEADS * HEAD_DIM
POOL_W = POOL_GROUPS * HEAD_DIM
SGU_W = SGU_HEADS * HEAD_DIM
D_MIX = CONV_W + POOL_W + SGU_W
D_IN = 3 * CONV_W + POOL_W + 2 * SGU_W
CONV_K = 3
POOL_WINDOWS = (2, 4, 8, 16)
CHUNK = 128
N_EXPERTS = 32
N_GROUPS = 4
EXPERTS_PER_GROUP = N_EXPERTS // N_GROUPS
TOP_K = 2
D_FF = D_MODEL // 2
ROW_BLOCK = 128
ADA_CHUNKS = 6
DEEPNORM_ALPHA = (2 * DEPTH) ** 0.25
DEEPNORM_BETA = (8 * DEPTH) ** -0.25
LN_EPS = 1e-5

kernel_name = "hybrid_conv_pool_sgu_grouped_moe_deepnorm"


def layer_norm(x, g, b):
    xf = x.astype(jnp.float32)
    mu = xf.mean(-1, keepdims=True)
    var = jnp.square(xf - mu).mean(-1, keepdims=True)
    return ((xf - mu) * lax.rsqrt(var + LN_EPS) * g + b).astype(x.dtype)


def short_conv(z, w, b):
    s = z.shape[1]
    zp = jnp.pad(z, ((0, 0), (CONV_K - 1, 0), (0, 0)))
    return w[0] * zp[:, 0:s] + w[1] * zp[:, 1:s + 1] + w[2] * zp[:, 2:s + 2] + b


def pool_mixer(p, w, scale):
    bsz, s, _ = p.shape
    cs = jnp.cumsum(p.astype(jnp.float32), axis=1)
    cs = jnp.pad(cs, ((0, 0), (1, 0), (0, 0)))
    t = jnp.arange(s)
    outs = []
    for gi, win in enumerate(POOL_WINDOWS):
        cg = cs[..., gi * HEAD_DIM:(gi + 1) * HEAD_DIM]
        upper = cg[:, 1:]
        lower = jnp.pad(cg[:, :s - win + 1], ((0, 0), (win - 1, 0), (0, 0)))
        count = jnp.minimum(t + 1, win).astype(jnp.float32)[None, :, None]
        outs.append((upper - lower) / count)
    pooled = jnp.concatenate(outs, axis=-1).astype(p.dtype) - p
    mixed = jnp.einsum('bsgc,gcd->bsgd', pooled.reshape(bsz, s, POOL_GROUPS, HEAD_DIM), w)
    return mixed.reshape(bsz, s, POOL_W) * scale


def spatial_gating(u, v, ln_g, ln_b, w_s, b_s):
    bsz, s, _ = v.shape
    v = layer_norm(v, ln_g, ln_b)
    vh = v.reshape(bsz, s // CHUNK, CHUNK, SGU_HEADS, HEAD_DIM)
    mask = jnp.tril(jnp.ones((CHUNK, CHUNK), dtype=bool))
    ws = jnp.where(mask[None], w_s, jnp.zeros_like(w_s))
    mixed = jnp.einsum('hts,bnshd->bnthd', ws, vh) + b_s.T[None, None, :, :, None]
    return u * mixed.reshape(bsz, s, SGU_W)


def token_mixer(h, w_in, conv_w, conv_b, pool_w, pool_scale, sgu_ln_g, sgu_ln_b, sgu_w, sgu_b, w_out):
    z = h @ w_in
    splits = [CONV_W, 2 * CONV_W, 3 * CONV_W, 3 * CONV_W + POOL_W, 3 * CONV_W + POOL_W + SGU_W]
    gb, gc, xc, p, u, v = jnp.split(z, splits, axis=-1)
    y_conv = gb * short_conv(gc * xc, conv_w, conv_b)
    y_pool = pool_mixer(p, pool_w, pool_scale)
    y_sgu = spatial_gating(u, v, sgu_ln_g, sgu_ln_b, sgu_w, sgu_b)
    y = jnp.concatenate([y_conv, y_pool, y_sgu], axis=-1)
    return y @ w_out


def expert_swiglu(args):
    xb, wg, wu, wd = args
    return (jax.nn.silu(xb @ wg) * (xb @ wu)) @ wd


def grouped_moe(h, w_router, b_router, w_gate, w_up, w_down):
    bsz, s, d = h.shape
    hf = h.reshape(-1, d)
    n_tok = hf.shape[0]
    n_assign = n_tok * TOP_K
    logits = (hf @ w_router + b_router).astype(jnp.float32)
    probs = jax.nn.softmax(logits, axis=-1)
    grouped = probs.reshape(n_tok, N_GROUPS, EXPERTS_PER_GROUP)
    group_score = lax.top_k(grouped, TOP_K)[0].sum(-1)
    g_sel = jnp.argmax(group_score, axis=-1)
    in_group = jnp.take_along_axis(grouped, g_sel[:, None, None], axis=1)[:, 0]
    top_p, top_i = lax.top_k(in_group, TOP_K)
    expert_idx = g_sel[:, None] * EXPERTS_PER_GROUP + top_i
    weights = top_p / top_p.sum(-1, keepdims=True)
    flat_e = expert_idx.reshape(-1).astype(jnp.int32)
    order = jnp.argsort(flat_e)
    sorted_e = flat_e[order]
    tok = order // TOP_K
    sizes = jnp.bincount(flat_e, length=N_EXPERTS).astype(jnp.int32)
    padded = ((sizes + ROW_BLOCK - 1) // ROW_BLOCK) * ROW_BLOCK
    pad_end = jnp.cumsum(padded)
    pad_start = pad_end - padded
    start = jnp.cumsum(sizes) - sizes
    rank = jnp.arange(n_assign, dtype=jnp.int32) - start[sorted_e]
    dest = pad_start[sorted_e] + rank
    n_blocks = n_assign // ROW_BLOCK + N_EXPERTS
    buf = jnp.zeros((n_blocks * ROW_BLOCK, d), h.dtype).at[dest].set(hf[tok])
    block_start = jnp.arange(n_blocks, dtype=jnp.int32) * ROW_BLOCK
    block_e = jnp.minimum(jnp.searchsorted(pad_end, block_start, side='right'), N_EXPERTS - 1)
    xb = buf.reshape(n_blocks, ROW_BLOCK, d)
    yb = lax.map(lambda a: expert_swiglu((a[0], w_gate[a[1]], w_up[a[1]], w_down[a[1]])), (xb, block_e))
    ys = yb.reshape(n_blocks * ROW_BLOCK, d)[dest]
    w_sorted = weights.reshape(-1)[order].astype(h.dtype)
    out = jnp.zeros_like(hf).at[tok].add(ys * w_sorted[:, None])
    return out.reshape(bsz, s, d)


def setup_inputs(seed: int = 0) -> dict:
    key = jax.random.key(seed)
    ks = jax.random.split(key, 24)
    f32 = jnp.float32
    nrm = lambda k, shape, sc: jax.random.normal(k, shape, f32) * sc
    L, D = DEPTH, D_MODEL
    return {
        "x": nrm(ks[0], (BATCH, SEQ, D), 1.0),
        "c": nrm(ks[1], (BATCH, D), 1.0),
        "w_ada": nrm(ks[2], (L, D, ADA_CHUNKS * D), 0.5 * D ** -0.5),
        "b_ada": nrm(ks[3], (L, ADA_CHUNKS * D), 0.01),
        "w_in": nrm(ks[4], (L, D, D_IN), D ** -0.5),
        "conv_w": nrm(ks[5], (L, CONV_K, CONV_W), CONV_K ** -0.5),
        "conv_b": nrm(ks[6], (L, CONV_W), 0.01),
        "pool_w": nrm(ks[7], (L, POOL_GROUPS, HEAD_DIM, HEAD_DIM), HEAD_DIM ** -0.5),
        "pool_scale": 1.0 + nrm(ks[8], (L, POOL_W), 0.1),
        "sgu_ln_g": 1.0 + nrm(ks[9], (L, SGU_W), 0.01),
        "sgu_ln_b": nrm(ks[10], (L, SGU_W), 0.01),
        "sgu_w": nrm(ks[11], (L, SGU_HEADS, CHUNK, CHUNK), CHUNK ** -0.5),
        "sgu_b": 1.0 + nrm(ks[12], (L, SGU_HEADS, CHUNK), 0.01),
        "w_out": nrm(ks[13], (L, D_MIX, D), DEEPNORM_BETA * D_MIX ** -0.5),
        "ln1_g": 1.0 + nrm(ks[14], (L, D), 0.01),
        "ln1_b": nrm(ks[15], (L, D), 0.01),
        "w_router": nrm(ks[16], (D, N_EXPERTS), D ** -0.5),
        "b_router": nrm(ks[17], (N_EXPERTS,), 0.01),
        "w_gate": nrm(ks[18], (L, N_EXPERTS, D, D_FF), D ** -0.5),
        "w_up": nrm(ks[19], (L, N_EXPERTS, D, D_FF), D ** -0.5),
        "w_down": nrm(ks[20], (L, N_EXPERTS, D_FF, D), DEEPNORM_BETA * D_FF ** -0.5),
        "ln2_g": 1.0 + nrm(ks[21], (L, D), 0.01),
        "ln2_b": nrm(ks[22], (L, D), 0.01),
    }


def reference(x, c, w_ada, b_ada, w_in, conv_w, conv_b, pool_w, pool_scale, sgu_ln_g, sgu_ln_b,
              sgu_w, sgu_b, w_out, ln1_g, ln1_b, w_router, b_router, w_gate, w_up, w_down,
              ln2_g, ln2_b):
    c_act = jax.nn.silu(c)
    for l in range(DEPTH):
        ada = c_act @ w_ada[l] + b_ada[l]
        sh1, sc1, g1, sh2, sc2, g2 = [a[:, None, :] for a in jnp.split(ada, ADA_CHUNKS, axis=-1)]
        h = x * (1.0 + sc1) + sh1
        y = token_mixer(h, w_in[l], conv_w[l], conv_b[l], pool_w[l], pool_scale[l],
                        sgu_ln_g[l], sgu_ln_b[l], sgu_w[l], sgu_b[l], w_out[l])
        x = layer_norm(DEEPNORM_ALPHA * x + g1 * y, ln1_g[l], ln1_b[l])
        h = x * (1.0 + sc2) + sh2
        y = grouped_moe(h, w_router, b_router, w_gate[l], w_up[l], w_down[l])
        x = layer_norm(DEEPNORM_ALPHA * x + g2 * y, ln2_g[l], ln2_b[l])
    return x
```

```python
import numpy as np
from contextlib import ExitStack
import concourse.bass as bass
import concourse.mybir as mybir
from concourse.bass_utils import run_bass_kernel_spmd

F32 = mybir.dt.float32
BF16 = mybir.dt.bfloat16
ALU = mybir.AluOpType
AF = mybir.ActivationFunctionType
AX = mybir.AxisListType

D = 1024
S = 2048
NT = S // 128
DEPTH = 4
D_IN = 2176
NE = 32
DFF = 512
JB = 8
CAP = JB * 128
NROWS = NE * CAP
I32 = mybir.dt.int32
ET = mybir.EngineType
ALPHA = float((2 * DEPTH) ** 0.25)
EPS = 1e-5


class Buf:
    def __init__(self, name, excl=False):
        self.name = name
        self.w = None
        self.r = {}
        self.excl = excl


class TR:
    def __init__(self, nc, es, ndma_sp=8, ndma_pool=8):
        self.nc = nc
        self.eng = {"pe": nc.tensor, "act": nc.scalar, "dve": nc.vector, "pool": nc.gpsimd, "sp": nc.sync}
        self.sem = {}
        self.cnt = {}
        for k in self.eng:
            self.sem[k] = es.enter_context(nc.semaphore("s_" + k))
            self.cnt[k] = 0
        self.dq = {"sp": [], "pool": []}
        for q, n in (("sp", ndma_sp), ("pool", ndma_pool)):
            for i in range(n):
                key = "d_%s%d" % (q, i)
                self.sem[key] = es.enter_context(nc.semaphore(key))
                self.cnt[key] = 0
                self.dq[q].append(key)
        self.dnext = {"sp": 0, "pool": 0}
        self.seen = {k: {} for k in self.eng}

    def wait(self, eng, dep):
        key, val = dep
        if key == "pe" and eng == "pe":
            return
        if self.seen[eng].get(key, 0) >= val:
            return
        self.eng[eng].wait_ge(self.sem[key], val)
        self.seen[eng][key] = val

    def _deps(self, reads, writes):
        deps = []
        for b in reads:
            if b.w is not None:
                deps.append(b.w)
            if b.excl:
                deps += list(b.r.items())
        for b in writes:
            if b.w is not None:
                deps.append(b.w)
            deps += list(b.r.items())
        best = {}
        for k, v in deps:
            if v > best.get(k, 0):
                best[k] = v
        return list(best.items())

    def _reg(self, tok, reads, writes):
        for b in reads:
            if b.excl:
                b.w = tok
                b.r = {}
            else:
                b.r[tok[0]] = max(b.r.get(tok[0], 0), tok[1])
        for b in writes:
            b.w = tok
            b.r = {}

    def op(self, eng, emits, reads=(), writes=()):
        for d in self._deps(reads, writes):
            self.wait(eng, d)
        if not isinstance(emits, (list, tuple)):
            emits = [emits]
        ins = None
        for f in emits:
            ins = f(self.eng[eng])
        self.cnt[eng] += 1
        ins.then_inc(self.sem[eng], 1)
        tok = (eng, self.cnt[eng])
        self._reg(tok, reads, writes)
        return tok

    def dma(self, q, out, in_, reads=(), writes=(), **kw):
        key = self.dq[q][self.dnext[q]]
        self.dnext[q] = (self.dnext[q] + 1) % len(self.dq[q])
        if self.cnt[key] > 0:
            self.wait(q, (key, 16 * self.cnt[key]))
        for d in self._deps(reads, writes):
            self.wait(q, d)
        ins = self.eng[q].dma_start(out=out, in_=in_, **kw)
        self.cnt[key] += 1
        ins.then_inc(self.sem[key], 16)
        tok = (key, 16 * self.cnt[key])
        self._reg(tok, reads, writes)
        return tok

    def idma(self, out, out_off, in_, in_off, bounds, reads=(), writes=()):
        q = "pool"
        key = self.dq[q][self.dnext[q]]
        self.dnext[q] = (self.dnext[q] + 1) % len(self.dq[q])
        if self.cnt[key] > 0:
            self.wait(q, (key, 16 * self.cnt[key]))
        for d in self._deps(reads, writes):
            self.wait(q, d)
        ins = self.nc.gpsimd.indirect_dma_start(
            out=out, out_offset=(bass.IndirectOffsetOnAxis(ap=out_off, axis=0) if out_off is not None else None),
            in_=in_, in_offset=(bass.IndirectOffsetOnAxis(ap=in_off, axis=0) if in_off is not None else None),
            bounds_check=bounds, oob_is_err=False)
        self.cnt[key] += 1
        ins.then_inc(self.sem[key], 16)
        tok = (key, 16 * self.cnt[key])
        self._reg(tok, reads, writes)
        return tok

    def cond_block(self, regs, j, body):
        before = dict(self.cnt)
        seen0 = {e: dict(d) for e, d in self.seen.items()}
        with self.nc.If_cmp(regs, j, "IS_GT"):
            body()
        after = dict(self.cnt)
        with self.nc.Else():
            for key in after:
                d = after[key] - before[key]
                if d == 0:
                    continue
                if key in self.eng:
                    owner, unit = key, 1
                else:
                    owner, unit = ("sp" if key.startswith("d_sp") else "pool"), 16
                if before[key] > 0:
                    self.eng[owner].wait_ge(self.sem[key], unit * before[key])
                self.eng[owner].sem_inc(self.sem[key], unit * d)
        self.seen = seen0

    def all_tokens(self):
        toks = []
        for k, c in self.cnt.items():
            if c > 0:
                toks.append((k, c if k in self.eng else 16 * c))
        return toks

    def barrier(self, engines=None):
        toks = self.all_tokens()
        for e in (engines or self.eng):
            for t in toks:
                if t[0] == e and e in ("pe",):
                    continue
                self.wait(e, t)


def build(n_layers=DEPTH):
    nc = bass.Bass("TRN2", target_bir_lowering=False)

    def din(name, shape, dt=F32):
        return nc.dram_tensor(name, list(shape), dt, kind="ExternalInput").ap()

    x_in = din("x", [S, D])
    cT_in = din("cT", [128, 8])
    w_ada = din("w_ada", [DEPTH, D, 6 * D])
    b_ada = din("b_ada", [DEPTH, 6 * D])
    w_in = din("w_in", [DEPTH, D, D_IN])
    cw_in = din("cw", [128, DEPTH * 3 * 4])
    pool_w = din("pool_w", [DEPTH, 4, 64, 64])
    psc_in = din("psc", [128, DEPTH * 2])
    sgu_ln_g = din("sgu_ln_g", [DEPTH, 384])
    sgu_ln_b = din("sgu_ln_b", [DEPTH, 384])
    sgu_wT = din("sgu_wT", [DEPTH, 6, 128, 128])
    sgu_b = din("sgu_b", [DEPTH, 6 * 128])
    w_out = din("w_out", [DEPTH, D, D])
    ln1_g = din("ln1_g", [DEPTH, D])
    ln1_b = din("ln1_b", [DEPTH, D])
    w_router = din("w_router", [D, NE])
    b_router = din("b_router", [1, NE])
    w_gate = din("w_gate", [DEPTH, NE, D, DFF])
    w_up = din("w_up", [DEPTH, NE, D, DFF])
    w_down = din("w_down", [DEPTH, NE, DFF, D])
    ln2_g = din("ln2_g", [DEPTH, D])
    ln2_b = din("ln2_b", [DEPTH, D])
    ident_in = din("ident", [128, 128])
    cmask_in = din("cmask", [128, 128])
    invw_in = din("invw", [128, 2])
    ic16_in = din("ic16", [128, 32])
    ltri_in = din("ltri", [128, 128])
    ecol_in = din("ecol", [128, 512])
    y_out = nc.dram_tensor("y", [S, D], F32, kind="ExternalOutput").ap()
    xs = nc.dram_tensor("xs", [S, D], F32, kind="Internal").ap()
    h2d = nc.dram_tensor("h2d", [S, D], BF16, kind="Internal").ap()
    xg = nc.dram_tensor("xg", [NROWS + 1, D], BF16, kind="Internal").ap()
    yg = nc.dram_tensor("yg", [NROWS + 1, D], F32, kind="Internal").ap()
    nblk_d = nc.dram_tensor("nblk_d", [1, NE], I32, kind="Internal").ap()

    es = ExitStack()
    with es:
        tr = TR(nc, es)

        uid = [0]

        def sb(scope, name, shape, dt=F32):
            uid[0] += 1
            return scope.enter_context(nc.sbuf_tensor("%s_u%d" % (name, uid[0]), list(shape), dt))

        banks = []
        for i in range(8):
            t = es.enter_context(nc.psum_tensor("ps%d" % i, [128, 512], F32))
            banks.append((t, Buf("ps%d" % i, excl=True)))
        bank_i = [0]

        def nb():
            b = banks[bank_i[0]]
            bank_i[0] = (bank_i[0] + 1) % 8
            return b

        hT = sb(es, "hT", [128, 8, S], BF16)
        hT_b = [Buf("hT%d" % i) for i in range(NT)]
        ident = sb(es, "ident", [128, 128], BF16)
        identf = sb(es, "identf", [128, 128], F32)
        cmask = sb(es, "cmask", [128, 128], BF16)
        ones_f = sb(es, "ones_f", [1, 128], F32)
        ones_b = sb(es, "ones_b", [1, 128], BF16)
        cb = sb(es, "cb", [128, 8, 128], F32)
        cbb = sb(es, "cbb", [128, 8, 128], BF16)
        cw = sb(es, "cw", [128, DEPTH * 12], F32)
        psc = sb(es, "psc", [128, DEPTH * 2], F32)
        invw = sb(es, "invw", [128, 2], F32)
        ic16 = sb(es, "ic16", [128, 32], F32)
        wr = sb(es, "wr", [128, 8, NE], BF16)
        brr = sb(es, "brr", [1, NE], BF16)
        cst = Buf("consts")
        ltri = sb(es, "ltri", [128, 128], BF16)
        ones128 = sb(es, "ones128", [128, 128], BF16)
        ecol = sb(es, "ecol", [128, 512], F32)
        h2d_b = [Buf("h2d%d" % i) for i in range(NT)]
        cregs = [nc.alloc_registers("nblk%d" % q, [ET.PE, ET.Activation, ET.DVE, ET.Pool, ET.SP]) for q in range(3)]

        with ExitStack() as s0:
            cT = sb(s0, "cT", [128, 8], F32)
            cact = sb(s0, "cact", [128, 8], F32)
            b_cT = Buf("cT")
            b_cact = Buf("cact")
            tr.dma("sp", cT[:], cT_in, writes=[b_cT])
            tr.dma("sp", cw[:], cw_in, writes=[cst])
            tr.dma("sp", psc[:], psc_in, writes=[cst])
            tr.dma("sp", invw[:], invw_in, writes=[cst])
            tr.dma("sp", ic16[:], ic16_in, writes=[cst])
            tr.dma("pool", ident[:], ident_in, writes=[cst])
            tr.dma("sp", identf[:], ident_in, writes=[cst])
            tr.dma("pool", cmask[:], cmask_in, writes=[cst])
            tr.dma("pool", wr[:], w_router.rearrange("(k p) n -> p k n", p=128), writes=[cst])
            tr.dma("pool", brr[:], b_router, writes=[cst])
            tr.dma("pool", ltri[:], ltri_in, writes=[cst])
            tr.dma("sp", ecol[:], ecol_in, writes=[cst])
            tr.op("dve", lambda e: e.memset(ones128[:], 1.0), writes=[cst])
            zrow = sb(s0, "zrow", [1, D], F32)
            b_z = Buf("zrow")
            tr.op("dve", lambda e: e.memset(zrow[:], 0.0), writes=[b_z])
            tr.dma("sp", yg[NROWS:NROWS + 1, :], zrow[:], reads=[b_z])
            tr.op("dve", lambda e: e.memset(ones_f[:], 1.0), writes=[cst])
            tr.op("dve", lambda e: e.memset(ones_b[:], 1.0), writes=[cst])
            tr.op("act", lambda e: e.activation(out=cact[:], in_=cT[:], func=AF.Silu), reads=[b_cT], writes=[b_cact])
            for k in range(8):
                tr.op("dve", lambda e, k=k: e.tensor_copy(out=cb[:, k, :], in_=cact[:, k:k + 1].to_broadcast([128, 128])),
                      reads=[b_cact], writes=[cst])
            tr.op("dve", lambda e: e.tensor_copy(out=cbb[:], in_=cb[:]), reads=[cst], writes=[cst])
            tr.barrier()

        def ada_chunk(scope_bufs, l, j, dst, dst_b, add_one):
            wa, wa_b, brow, brow_b, wab, wab_b = scope_bufs
            for half in range(2):
                c0 = j * 1024 + half * 512
                i = (j * 2 + half) % 2
                tr.dma("sp", wa[i][:], w_ada[l].rearrange("(k p) n -> p k n", p=128)[:, :, c0:c0 + 512],
                       writes=[wa_b[i]])
                tr.op("dve", lambda e, i=i: e.tensor_copy(out=wab[i][:], in_=wa[i][:]), reads=[wa_b[i]], writes=[wab_b[i]])
                bt, bb = nb()
                em = []
                for k in range(8):
                    em.append(lambda e, k=k, i=i, bt=bt: e.matmul(bt[:, :], lhsT=cbb[:, k, :], rhs=wab[i][:, k, :],
                                                                  start=(k == 0), stop=False))
                em.append(lambda e, bt=bt, c0=c0: e.matmul(bt[:, :], lhsT=ones_f[0:1, :], rhs=brow[0:1, c0:c0 + 512],
                                                           start=False, stop=True))
                tr.op("pe", em, reads=[wab_b[i], brow_b, cst], writes=[bb])
                tr.op("act", lambda e, bt=bt, half=half: e.activation(
                    out=dst[:, half * 512:(half + 1) * 512], in_=bt[:, :], func=AF.Identity,
                    bias=(1.0 if add_one else 0.0)), reads=[bb], writes=[dst_b])

        def bcast_load(dst, dst_b, src_row):
            tr.dma("sp", dst[:], src_row.partition_broadcast(128), writes=[dst_b])

        def ln_stats(scope_t, u, u_b, width, nchunk):
            st, mv, rs, nmr, b_st = scope_t
            em = []
            cwid = width // nchunk
            for c in range(nchunk):
                em.append(lambda e, c=c: e.bn_stats(out=st[:, c, :], in_=u[:, c * cwid:(c + 1) * cwid]))
            tr.op("dve", em, reads=[u_b], writes=[b_st[0]])
            tr.op("dve", lambda e: e.bn_aggr(out=mv[:, :], in_=st[:, 0:nchunk, :]), reads=[b_st[0]], writes=[b_st[1]])

        def ln_rstd(scope_t):
            st, mv, rs, nmr, b_st = scope_t
            tr.op("act", lambda e: e.activation(out=nmr[:, :], in_=mv[:, 1:2], func=AF.Sqrt, bias=EPS),
                  reads=[b_st[1]], writes=[b_st[3]])
            tr.op("dve", lambda e: e.reciprocal(out=rs[:, :], in_=nmr[:, :]), reads=[b_st[3]], writes=[b_st[2]])
            tr.op("dve", lambda e: e.scalar_tensor_tensor(out=nmr[:, :], in0=mv[:, 0:1], scalar=-1.0, in1=rs[:, :],
                                                          op0=ALU.mult, op1=ALU.mult),
                  reads=[b_st[1], b_st[2]], writes=[b_st[3]])
            return rs, nmr, [b_st[2], b_st[3]]

        def ln_sqrt(scope_t):
            st, mv, rs, nmr, b_st = scope_t
            tr.op("act", lambda e: e.activation(out=nmr[:, :], in_=mv[:, 1:2], func=AF.Sqrt, bias=EPS),
                  reads=[b_st[1]], writes=[b_st[3]])

        def ln_rs(scope_t):
            st, mv, rs, nmr, b_st = scope_t
            tr.op("dve", lambda e: e.reciprocal(out=rs[:, :], in_=nmr[:, :]), reads=[b_st[3]], writes=[b_st[2]])
            tr.op("dve", lambda e: e.scalar_tensor_tensor(out=nmr[:, :], in0=mv[:, 0:1], scalar=-1.0, in1=rs[:, :],
                                                          op0=ALU.mult, op1=ALU.mult),
                  reads=[b_st[1], b_st[2]], writes=[b_st[3]])
            return rs, nmr, [b_st[2], b_st[3]]

        def ln_tile(scope_t, u, u_b, width, nchunk):
            ln_stats(scope_t, u, u_b, width, nchunk)
            return ln_rstd(scope_t)

        def ln_bufs(scope, n):
            out = []
            for i in range(n):
                out.append((sb(scope, "st%d" % i, [128, 2, 6]), sb(scope, "mv%d" % i, [128, 2]),
                            sb(scope, "rs%d" % i, [128, 1]), sb(scope, "nmr%d" % i, [128, 1]),
                            [Buf("lnst%d_%d" % (i, q)) for q in range(4)]))
            return out

        def pipeline(n, stages, name=""):
            import os
            if name and name in os.environ.get("PIPE_OFF", ""):
                for it in range(n):
                    for f in stages:
                        f(it)
                return
            for step in range(n + len(stages) - 1):
                for si, f in enumerate(stages):
                    it = step - si
                    if 0 <= it < n:
                        f(it)

        def transpose_to_hT(src, src_b, tt):
            bt, bb = nb()
            btb = bt.bitcast(BF16)
            em = []
            for k in range(8):
                em.append(lambda e, k=k: e.transpose(btb[:, k * 128:(k + 1) * 128], src[:, k * 128:(k + 1) * 128], ident[:]))
            tr.op("pe", em, reads=[src_b, cst], writes=[bb])
            tr.op("act", lambda e: e.activation(out=hT[:, :, tt * 128:(tt + 1) * 128],
                                                in_=btb[:, 0:1024].rearrange("p (k t) -> p k t", k=8),
                                                func=AF.Identity), reads=[bb], writes=[hT_b[tt]])

        x_src = x_in
        for l in range(n_layers):
            last = (l == n_layers - 1)
            x_dst = y_out if last else xs
            with ExitStack() as pa:
                M1 = sb(pa, "M1", [128, D]); S1 = sb(pa, "S1", [128, D]); G1 = sb(pa, "G1", [128, D])
                M2 = sb(pa, "M2", [128, D]); S2 = sb(pa, "S2", [128, D])
                LG = sb(pa, "LG1", [128, D]); LB = sb(pa, "LB1", [128, D])
                bM1, bS1, bG1, bM2, bS2, bLG, bLB = [Buf(n) for n in ("M1", "S1", "G1", "M2", "S2", "LG", "LB")]
                with ExitStack() as pt:
                    wa = [sb(pt, "wa%d" % i, [128, 8, 512]) for i in range(2)]
                    wa_b = [Buf("wa0"), Buf("wa1")]
                    brow = sb(pt, "brow", [1, 6 * D]); brow_b = Buf("brow")
                    tr.dma("sp", brow[:], b_ada[l:l + 1, :], writes=[brow_b])
                    wab = [sb(pt, "wab%d" % i, [128, 8, 512], BF16) for i in range(2)]
                    wab_b = [Buf("wab0"), Buf("wab1")]
                    sc = (wa, wa_b, brow, brow_b, wab, wab_b)
                    ada_chunk(sc, l, 0, S1, bS1, False)
                    ada_chunk(sc, l, 1, M1, bM1, True)
                    ada_chunk(sc, l, 2, G1, bG1, False)
                    ada_chunk(sc, l, 3, S2, bS2, False)
                    ada_chunk(sc, l, 4, M2, bM2, True)
                    bcast_load(LG, bLG, ln1_g[l:l + 1, :])
                    bcast_load(LB, bLB, ln1_b[l:l + 1, :])
                    tr.barrier()

                yT = sb(pa, "yT", [128, 8, S], BF16)
                yT_b = [Buf("yT%d" % i) for i in range(8)]
                xt = [sb(pa, "xt%d" % i, [128, D]) for i in range(3)]
                xt_b = [Buf("xt0"), Buf("xt1"), Buf("xt2")]
                tmp = [sb(pa, "tmp%d" % i, [128, D]) for i in range(2)]
                tmp_b = [Buf("tmp0"), Buf("tmp1")]
                hb = [sb(pa, "hb%d" % i, [128, D], BF16) for i in range(2)]
                hb_b = [Buf("hb0"), Buf("hb1")]

                def a1_ld(tt):
                    tr.dma("sp", xt[tt % 3][:], x_src[tt * 128:(tt + 1) * 128, :], writes=[xt_b[tt % 3]])

                def a1_s0(tt):
                    i = tt % 2
                    tr.op("dve", lambda e: e.tensor_tensor(out=tmp[i][:], in0=xt[tt % 3][:], in1=M1[:], op=ALU.mult),
                          reads=[xt_b[tt % 3], bM1], writes=[tmp_b[i]])
                    tr.op("pool" if tt % 3 == 2 else "dve",
                          lambda e: e.tensor_tensor(out=hb[i][:], in0=tmp[i][:], in1=S1[:], op=ALU.add),
                          reads=[tmp_b[i], bS1], writes=[hb_b[i]])

                def a1_s1(tt):
                    transpose_to_hT(hb[tt % 2], hb_b[tt % 2], tt)
                pipeline(NT, [a1_ld, a1_s0, a1_s1], "A1")

                with ExitStack() as p2:
                    W = [sb(p2, "W%d" % i, [128, S + 16]) for i in range(4)]
                    W_b = [Buf("W%d" % i) for i in range(4)]
                    wc = [sb(p2, "wc%d" % i, [128, 8, 128], BF16) for i in range(4)]
                    wc_b = [Buf("wc%d" % i) for i in range(4)]
                    wci = [0]
                    pbf = sb(p2, "pbf", [128, S], BF16); pbf_b = Buf("pbf")
                    pwbd = sb(p2, "pwbd", [128, 2, 128], BF16); pwbd_b = Buf("pwbd")
                    wv = sb(p2, "wv", [128, 8, 384], BF16); wv_b = Buf("wv")
                    vnb = sb(p2, "vnb", [128, NT, 384], BF16); vnb_b = [Buf("vnb%d" % i) for i in range(NT)]
                    SG = sb(p2, "SG", [128, 384]); SBt = sb(p2, "SBt", [128, 384]); bSG = Buf("SG"); bSB = Buf("SB")
                    wsm = sb(p2, "wsm", [128, 6, 128], BF16); wsm_b = Buf("wsm")
                    wsr = sb(p2, "wsr", [128, 6, 128], BF16); wsr_b = Buf("wsr")
                    bsr = sb(p2, "bsr", [1, 6 * 128], BF16); bsr_b = Buf("bsr")
                    vt = [sb(p2, "vt%d" % i, [128, 384]) for i in range(2)]; vt_b = [Buf("vt0"), Buf("vt1")]
                    vt2 = [sb(p2, "vu%d" % i, [128, 384]) for i in range(2)]; vt2_b = [Buf("vu0"), Buf("vu1")]
                    lnt = ln_bufs(p2, 4)

                    w_in_l = w_in[l].rearrange("(k p) n -> p k n", p=128)

                    def zmm(f):
                        i = wci[0]
                        wci[0] = (wci[0] + 1) % 4
                        tr.dma("pool", wc[i][:], w_in_l[:, :, f * 128:(f + 1) * 128], writes=[wc_b[i]])
                        out = []
                        for tg in range(4):
                            bt, bb = nb()
                            em = [lambda e, k=k, bt=bt, tg=tg: e.matmul(bt[:, :], lhsT=wc[i][:, k, :],
                                                                        rhs=hT[:, k, tg * 512:(tg + 1) * 512],
                                                                        start=(k == 0), stop=(k == 7)) for k in range(8)]
                            tr.op("pe", em, reads=[wc_b[i]] + hT_b[tg * 4:(tg + 1) * 4], writes=[bb])
                            out.append((bt, bb))
                        return out

                    for j in range(3):
                        cwo = (l * 3 + j) * 4
                        tr.op("dve", lambda e: e.memset(W[1][:, 0:2], 0.0), writes=[W_b[1]])
                        bk = zmm(3 + j)
                        for tg in range(4):
                            tr.op("act", lambda e, tg=tg, bt=bk[tg][0]: e.activation(
                                out=W[0][:, tg * 512:(tg + 1) * 512], in_=bt[:, :], func=AF.Identity),
                                reads=[bk[tg][1]], writes=[W_b[0]])
                        bk = zmm(6 + j)
                        for tg in range(4):
                            tr.op("dve", lambda e, tg=tg, bt=bk[tg][0]: e.tensor_tensor(
                                out=W[1][:, 2 + tg * 512:2 + (tg + 1) * 512], in0=W[0][:, tg * 512:(tg + 1) * 512],
                                in1=bt[:, :], op=ALU.mult), reads=[W_b[0], bk[tg][1]], writes=[W_b[1]])
                        tr.op("dve", lambda e: e.tensor_scalar(out=W[2][:, 0:S], in0=W[1][:, 2:2 + S],
                                                               scalar1=cw[:, cwo + 2:cwo + 3], scalar2=cw[:, cwo + 3:cwo + 4],
                                                               op0=ALU.mult, op1=ALU.add),
                              reads=[W_b[1], cst], writes=[W_b[2]])
                        tr.op("dve", lambda e: e.scalar_tensor_tensor(out=W[3][:, 0:S], in0=W[1][:, 1:1 + S],
                                                                      scalar=cw[:, cwo + 1:cwo + 2], in1=W[2][:, 0:S],
                                                                      op0=ALU.mult, op1=ALU.add),
                              reads=[W_b[1], W_b[2], cst], writes=[W_b[3]])
                        tr.op("dve", lambda e: e.scalar_tensor_tensor(out=W[2][:, 0:S], in0=W[1][:, 0:S],
                                                                      scalar=cw[:, cwo:cwo + 1], in1=W[3][:, 0:S],
                                                                      op0=ALU.mult, op1=ALU.add),
                              reads=[W_b[1], W_b[3], cst], writes=[W_b[2]])
                        bk = zmm(j)
                        for tg in range(4):
                            tr.op("dve", lambda e, tg=tg, bt=bk[tg][0]: e.tensor_tensor(
                                out=yT[:, j, tg * 512:(tg + 1) * 512], in0=W[2][:, tg * 512:(tg + 1) * 512],
                                in1=bt[:, :], op=ALU.mult), reads=[W_b[2], bk[tg][1]], writes=[yT_b[j]])

                    tr.op("dve", lambda e: e.memset(pwbd[:], 0.0), writes=[pwbd_b])
                    for g in range(4):
                        o = (g % 2) * 64
                        tr.dma("pool", pwbd[o:o + 64, g // 2, o:o + 64], pool_w[l, g], writes=[pwbd_b])
                    for c in range(2):
                        for i in range(4):
                            tr.op("dve", lambda e, i=i: e.memset(W[i][:, 0:16], 0.0), writes=[W_b[i]])
                        bk = zmm(9 + c)
                        for tg in range(4):
                            tr.op("act", lambda e, tg=tg, bt=bk[tg][0]: e.activation(
                                out=W[0][:, 16 + tg * 512:16 + (tg + 1) * 512], in_=bt[:, :], func=AF.Identity),
                                reads=[bk[tg][1]], writes=[W_b[0]])

                        def lvl(dst, src, sh):
                            tr.op("dve", lambda e: e.tensor_tensor(out=W[dst][:, 16:16 + S], in0=W[src][:, 16:16 + S],
                                                                   in1=W[src][:, 16 - sh:16 - sh + S], op=ALU.add),
                                  reads=[W_b[src]], writes=[W_b[dst]])
                        lvl(1, 0, 1)
                        lvl(2, 1, 2)
                        if c == 0:
                            lo, hi, pt_ = 1, 2, 3
                        else:
                            lvl(3, 2, 4)
                            lvl(1, 3, 8)
                            lo, hi, pt_ = 3, 1, 2
                        for (sel, p0) in ((lo, 0), (hi, 64)):
                            tr.op("dve", [lambda e, sel=sel, p0=p0: e.tensor_scalar(
                                out=W[pt_][p0:p0 + 64, 16:16 + S], in0=W[sel][p0:p0 + 64, 16:16 + S],
                                scalar1=invw[p0:p0 + 64, c:c + 1], scalar2=None, op0=ALU.mult)],
                                reads=[W_b[sel], cst], writes=[W_b[pt_]])
                        for (sel, p0) in ((lo, 0), (hi, 64)):
                            tr.op("dve", [lambda e, sel=sel, p0=p0: e.tensor_tensor(
                                out=W[pt_][p0:p0 + 64, 16:32], in0=W[sel][p0:p0 + 64, 16:32],
                                in1=ic16[p0:p0 + 64, c * 16:(c + 1) * 16], op=ALU.mult)],
                                reads=[W_b[sel], cst], writes=[W_b[pt_]])
                        tr.op("dve", lambda e: e.tensor_tensor(out=pbf[:, :], in0=W[pt_][:, 16:16 + S],
                                                               in1=W[0][:, 16:16 + S], op=ALU.subtract),
                              reads=[W_b[pt_], W_b[0]], writes=[pbf_b])
                        for tg in range(4):
                            bt, bb = nb()
                            tr.op("pe", lambda e, bt=bt, tg=tg: e.matmul(bt[:, :], lhsT=pwbd[:, c, :],
                                                                         rhs=pbf[:, tg * 512:(tg + 1) * 512],
                                                                         start=True, stop=True),
                                  reads=[pwbd_b, pbf_b], writes=[bb])
                            tr.op("act", lambda e, bt=bt, tg=tg: e.activation(
                                out=yT[:, 3 + c, tg * 512:(tg + 1) * 512], in_=bt[:, :], func=AF.Identity,
                                scale=psc[:, l * 2 + c:l * 2 + c + 1]), reads=[bb, cst], writes=[yT_b[3 + c]])

                    tr.dma("pool", wv[:], w_in_l[:, :, 1792:2176], writes=[wv_b])
                    bcast_load(SG, bSG, sgu_ln_g[l:l + 1, :])
                    bcast_load(SBt, bSB, sgu_ln_b[l:l + 1, :])
                    tr.dma("pool", wsr[:], sgu_wT[l].rearrange("h s t -> s h t"), writes=[wsr_b])
                    tr.dma("pool", bsr[:], sgu_b[l:l + 1, :], writes=[bsr_b])
                    for h in range(6):
                        tr.op("dve", lambda e, h=h: e.tensor_tensor(out=wsm[:, h, :], in0=wsr[:, h, :], in1=cmask[:],
                                                                    op=ALU.mult), reads=[wsr_b, cst], writes=[wsm_b])
                    vbank = {}
                    vrs = {}

                    def v_s0(tt):
                        bt, bb = nb()
                        em = [lambda e, k=k: e.matmul(bt[:, 0:384], lhsT=hT[:, k, tt * 128:(tt + 1) * 128],
                                                      rhs=wv[:, k, :], start=(k == 0), stop=(k == 7))
                              for k in range(8)]
                        tr.op("pe", em, reads=[wv_b, hT_b[tt]], writes=[bb])
                        ln_stats(lnt[tt % 4], bt[:, 0:384], bb, 384, 1)
                        vbank[tt] = (bt, bb)

                    def v_s1(tt):
                        vrs[tt] = ln_rstd(lnt[tt % 4])

                    def v_s2(tt):
                        i = tt % 2
                        bt, bb = vbank.pop(tt)
                        rs, nmr, sb_ = vrs.pop(tt)
                        tr.op("act", lambda e: e.activation(
                            out=vt[i][:], in_=bt[:, 0:384], func=AF.Identity, scale=rs[:, 0:1], bias=nmr[:, 0:1]),
                            reads=[bb] + sb_, writes=[vt_b[i]])
                        tr.op("dve", lambda e: e.tensor_tensor(out=vt2[i][:], in0=vt[i][:], in1=SG[:], op=ALU.mult),
                              reads=[vt_b[i], bSG], writes=[vt2_b[i]])
                        tr.op("dve", lambda e: e.tensor_tensor(out=vnb[:, tt, :], in0=vt2[i][:], in1=SBt[:], op=ALU.add),
                              reads=[vt2_b[i], bSB], writes=[vnb_b[tt]])
                    pipeline(NT, [v_s0, v_s1, v_s2], "SV")
                    for pr in range(3):
                        bk = zmm(11 + pr)
                        for tg in range(4):
                            tr.op("act", lambda e, tg=tg, bt=bk[tg][0]: e.activation(
                                out=W[0][:, tg * 512:(tg + 1) * 512], in_=bt[:, :], func=AF.Identity),
                                reads=[bk[tg][1]], writes=[W_b[0]])
                        for hh in range(2):
                            h = 2 * pr + hh
                            p0 = hh * 64
                            for tg in range(4):
                                bt, bb = nb()
                                em = []
                                for q in range(4):
                                    tt = tg * 4 + q
                                    em.append(lambda e, bt=bt, q=q, tt=tt: e.matmul(
                                        bt[:, q * 128:(q + 1) * 128], lhsT=vnb[:, tt, pr * 128:(pr + 1) * 128],
                                        rhs=wsm[:, h, :], start=True, stop=False))
                                    em.append(lambda e, bt=bt, q=q: e.matmul(
                                        bt[:, q * 128:(q + 1) * 128], lhsT=ones_b[0:1, :],
                                        rhs=bsr[0:1, h * 128:(h + 1) * 128], start=False, stop=True))
                                tr.op("pe", em, reads=[wsm_b, bsr_b, cst] + vnb_b[tg * 4:(tg + 1) * 4], writes=[bb])
                                tr.op("dve", lambda e, bt=bt, tg=tg: e.tensor_tensor(
                                    out=yT[p0:p0 + 64, 5 + pr, tg * 512:(tg + 1) * 512],
                                    in0=W[0][p0:p0 + 64, tg * 512:(tg + 1) * 512], in1=bt[p0:p0 + 64, :], op=ALU.mult),
                                    reads=[W_b[0], bb], writes=[yT_b[5 + pr]])
                    tr.barrier()

                with ExitStack() as p3:
                    wo = sb(p3, "wo", [128, 8, D], BF16); wo_b = Buf("wo")
                    tr.dma("pool", wo[:], w_out[l].rearrange("(k p) n -> p k n", p=128), writes=[wo_b])
                    for k in range(8):
                        tr.op("dve" if k % 2 == 0 else "pool", lambda e, k=k: e.tensor_tensor(
                            out=wo[:, k, :], in0=wo[:, k, :], in1=G1[:], op=ALU.mult), reads=[wo_b, bG1], writes=[wo_b])
                    GM = sb(p3, "GM", [128, D]); BM = sb(p3, "BM", [128, D]); bGM = Buf("GM"); bBM = Buf("BM")
                    tr.op("dve", lambda e: e.tensor_tensor(out=GM[:], in0=LG[:], in1=M2[:], op=ALU.mult),
                          reads=[bLG, bM2], writes=[bGM])
                    tr.op("dve", lambda e: e.tensor_tensor(out=BM[:], in0=LB[:], in1=M2[:], op=ALU.mult),
                          reads=[bLB, bM2], writes=[bBM])
                    tr.op("pool", lambda e: e.tensor_tensor(out=BM[:], in0=BM[:], in1=S2[:], op=ALU.add),
                          reads=[bBM, bS2], writes=[bBM])
                    uu = [sb(p3, "uu%d" % i, [128, D]) for i in range(4)]; uu_b = [Buf("uu%d" % i) for i in range(4)]
                    xn = [sb(p3, "xn%d" % i, [128, D]) for i in range(2)]; xn_b = [Buf("xna"), Buf("xnb")]
                    x1 = [sb(p3, "x1%d" % i, [128, D]) for i in range(2)]; x1_b = [Buf("x1a"), Buf("x1b")]
                    tb = [sb(p3, "tb%d" % i, [128, D]) for i in range(2)]; tb_b = [Buf("tba"), Buf("tbb")]
                    lnt = ln_bufs(p3, 4)

                    def a3_ld(tt):
                        tr.dma("sp", xt[tt % 3][:], x_src[tt * 128:(tt + 1) * 128, :], writes=[xt_b[tt % 3]])

                    def a3_sA(tt):
                        i = tt % 2
                        i4 = tt % 4
                        for half in range(2):
                            bt, bb = nb()
                            em = [lambda e, k=k, bt=bt, half=half: e.matmul(
                                bt[:, :], lhsT=yT[:, k, tt * 128:(tt + 1) * 128], rhs=wo[:, k, half * 512:(half + 1) * 512],
                                start=(k == 0), stop=(k == 7)) for k in range(8)]
                            tr.op("pe", em, reads=[wo_b] + yT_b, writes=[bb])
                            tr.op("dve", lambda e, bt=bt, half=half: e.scalar_tensor_tensor(
                                out=uu[i4][:, half * 512:(half + 1) * 512], in0=xt[tt % 3][:, half * 512:(half + 1) * 512],
                                scalar=ALPHA, in1=bt[:, :], op0=ALU.mult, op1=ALU.add),
                                reads=[xt_b[tt % 3], bb], writes=[uu_b[i4]])
                        ln_stats(lnt[i4], uu[i4], uu_b[i4], D, 2)

                    rsn = {}
                    tbank = {}

                    def a3_s1(tt):
                        ln_sqrt(lnt[tt % 4])

                    def a3_s2(tt):
                        rsn[tt] = ln_rs(lnt[tt % 4])

                    def a3_s3(tt):
                        i = tt % 2
                        i4 = tt % 4
                        rs, nmr, sb_ = rsn.pop(tt)
                        tr.op("act", lambda e: e.activation(out=xn[i][:], in_=uu[i4][:], func=AF.Identity,
                                                            scale=rs[:, 0:1], bias=nmr[:, 0:1]),
                              reads=[uu_b[i4]] + sb_, writes=[xn_b[i]])

                    def a3_s4(tt):
                        i = tt % 2
                        tr.op("dve", lambda e: e.tensor_tensor(out=tmp[i][:], in0=xn[i][:], in1=LG[:], op=ALU.mult),
                              reads=[xn_b[i], bLG], writes=[tmp_b[i]])
                        tr.op("dve", lambda e: e.tensor_tensor(out=x1[i][:], in0=tmp[i][:], in1=LB[:], op=ALU.add),
                              reads=[tmp_b[i], bLB], writes=[x1_b[i]])
                        tr.dma("pool", xs[tt * 128:(tt + 1) * 128, :], x1[i][:], reads=[x1_b[i]])
                        tr.op("dve", lambda e: e.tensor_tensor(out=tb[i][:], in0=xn[i][:], in1=GM[:], op=ALU.mult),
                              reads=[xn_b[i], bGM], writes=[tb_b[i]])
                        tr.op("pool", lambda e: e.tensor_tensor(out=hb[i][:], in0=tb[i][:], in1=BM[:], op=ALU.add),
                              reads=[tb_b[i], bBM], writes=[hb_b[i]])
                        tr.dma("pool", h2d[tt * 128:(tt + 1) * 128, :], hb[i][:], reads=[hb_b[i]], writes=[h2d_b[tt]])

                    def a3_s5(tt):
                        i = tt % 2
                        bt, bb = nb()
                        btb = bt.bitcast(BF16)
                        em = [lambda e, k=k: e.transpose(btb[:, k * 128:(k + 1) * 128], hb[i][:, k * 128:(k + 1) * 128],
                                                         ident[:]) for k in range(8)]
                        tr.op("pe", em, reads=[hb_b[i], cst], writes=[bb])
                        tbank[tt] = (btb, bb)

                    def a3_s6(tt):
                        btb, bb = tbank.pop(tt)
                        tr.op("act", lambda e: e.activation(out=hT[:, :, tt * 128:(tt + 1) * 128],
                                                            in_=btb[:, 0:1024].rearrange("p (k t) -> p k t", k=8),
                                                            func=AF.Identity), reads=[bb], writes=[hT_b[tt]])
                    pipeline(NT, [a3_ld, a3_sA, a3_s1, a3_s2, a3_s3, a3_s4, a3_s5, a3_s6], "A3")
                    tr.barrier()

            with ExitStack() as pb:
                G2 = sb(pb, "G2", [128, D]); LG = sb(pb, "LG2", [128, D]); LB = sb(pb, "LB2", [128, D])
                bG2, bLG, bLB = Buf("G2"), Buf("LG2"), Buf("LB2")
                with ExitStack() as pt:
                    wa = [sb(pt, "wa%d" % i, [128, 8, 512]) for i in range(2)]
                    wa_b = [Buf("wa0"), Buf("wa1")]
                    brow = sb(pt, "brow", [1, 6 * D]); brow_b = Buf("brow")
                    tr.dma("sp", brow[:], b_ada[l:l + 1, :], writes=[brow_b])
                    wab = [sb(pt, "wab%d" % i, [128, 8, 512], BF16) for i in range(2)]
                    wab_b = [Buf("wab0"), Buf("wab1")]
                    ada_chunk((wa, wa_b, brow, brow_b, wab, wab_b), l, 5, G2, bG2, False)
                    bcast_load(LG, bLG, ln2_g[l:l + 1, :])
                    bcast_load(LB, bLB, ln2_b[l:l + 1, :])
                    tr.barrier()
                idx_hi = sb(pb, "idx_hi", [128, 16], I32); idx_lo = sb(pb, "idx_lo", [128, 16], I32)
                whi = sb(pb, "whi", [128, 16]); wlo = sb(pb, "wlo", [128, 16])
                idx_b = Buf("idx"); wgt_b = Buf("wgt")
                yg_b = [Buf("yg%d" % i) for i in range(NE)]
                with ExitStack() as pm:
                    wg = [sb(pm, "wg%d" % i, [128, 8, DFF], BF16) for i in range(2)]
                    wu = [sb(pm, "wu%d" % i, [128, 8, DFF], BF16) for i in range(2)]
                    wd = [sb(pm, "wd%d" % i, [128, 4, D], F32) for i in range(2)]
                    wg_b = [Buf("wg0"), Buf("wg1")]; wu_b = [Buf("wu0"), Buf("wu1")]; wd_b = [Buf("wd0"), Buf("wd1")]
                    sl = [sb(pm, "sl%d" % i, [128, 512]) for i in range(2)]; sl_b = [Buf("sla"), Buf("slb")]
                    RL = sb(pm, "RL", [128, 512]); RE = sb(pm, "RE", [128, 512]); RT = sb(pm, "RT", [128, 512])
                    RM = sb(pm, "RM", [128, 512]); Wt = sb(pm, "Wt", [128, 512])
                    r16 = sb(pm, "r16", [128, 16]); g16 = sb(pm, "g16", [128, 16]); rg16 = sb(pm, "rg16", [128, 16])
                    m1 = sb(pm, "m1", [128, 64]); m2 = sb(pm, "m2", [128, 64]); scg = sb(pm, "scg", [128, 64])
                    gs = sb(pm, "gs", [128, 64])
                    rb = Buf("route")
                    Wt_b = Buf("Wt")

                    def wprefetch(ex):
                        i = ex % 2
                        tr.dma("pool", wg[i][:], w_gate[l, ex].rearrange("(p k) n -> p k n", p=128), writes=[wg_b[i]])
                        tr.dma("pool", wu[i][:], w_up[l, ex].rearrange("(p k) n -> p k n", p=128), writes=[wu_b[i]])
                        tr.dma("sp", wd[i][:], w_down[l, ex].rearrange("(p k) n -> p k n", p=128), writes=[wd_b[i]])
                    wprefetch(0)
                    wprefetch(1)

                    bt, bb = nb()
                    em = []
                    for tt in range(NT):
                        for k in range(8):
                            em.append(lambda e, k=k, tt=tt: e.matmul(bt[:, tt * 32:(tt + 1) * 32],
                                                                     lhsT=hT[:, k, tt * 128:(tt + 1) * 128], rhs=wr[:, k, :],
                                                                     start=(k == 0), stop=False))
                        em.append(lambda e, tt=tt: e.matmul(bt[:, tt * 32:(tt + 1) * 32], lhsT=ones_b[0:1, :], rhs=brr[0:1, :],
                                                            start=False, stop=True))
                    tr.op("pe", em, reads=hT_b + [cst], writes=[bb])
                    tr.op("act", lambda e: e.activation(out=RL[:], in_=bt[:, :], func=AF.Identity), reads=[bb], writes=[rb])

                    def v3(t, a, b):
                        return t[:, :].rearrange("p (a b) -> p a b", a=a)

                    def bc3(t, a, b):
                        return t[:, :].unsqueeze(2).to_broadcast([128, a, b])

                    def rop(f):
                        tr.op("dve", f, reads=[rb], writes=[rb])
                    rop(lambda e: e.tensor_reduce(out=r16[:, :], in_=v3(RL, 16, 32), axis=AX.X, op=ALU.max))
                    rop(lambda e: e.tensor_tensor(out=v3(RT, 16, 32), in0=v3(RL, 16, 32), in1=bc3(r16, 16, 32), op=ALU.subtract))
                    tr.op("act", lambda e: e.activation(out=RE[:], in_=RT[:], func=AF.Exp), reads=[rb], writes=[rb])
                    rop(lambda e: e.tensor_reduce(out=m1[:, :], in_=v3(RE, 64, 8), axis=AX.X, op=ALU.max))
                    rop(lambda e: e.tensor_tensor(out=v3(RT, 64, 8), in0=v3(RE, 64, 8), in1=bc3(m1, 64, 8), op=ALU.is_equal))
                    rop(lambda e: e.tensor_tensor(out=RT[:], in0=RT[:], in1=RE[:], op=ALU.mult))
                    rop(lambda e: e.tensor_tensor(out=RM[:], in0=RE[:], in1=RT[:], op=ALU.subtract))
                    rop(lambda e: e.tensor_reduce(out=m2[:, :], in_=v3(RM, 64, 8), axis=AX.X, op=ALU.max))
                    rop(lambda e: e.tensor_tensor(out=scg[:], in0=m1[:], in1=m2[:], op=ALU.add))
                    rop(lambda e: e.tensor_reduce(out=g16[:, :], in_=v3(scg, 16, 4), axis=AX.X, op=ALU.max))
                    rop(lambda e: e.tensor_tensor(out=v3(gs, 16, 4), in0=v3(scg, 16, 4), in1=bc3(g16, 16, 4), op=ALU.is_equal))
                    rop(lambda e: e.tensor_tensor(out=v3(RT, 64, 8), in0=v3(RE, 64, 8), in1=bc3(m2, 64, 8), op=ALU.is_ge))
                    rop(lambda e: e.tensor_tensor(out=v3(RM, 64, 8), in0=v3(RT, 64, 8), in1=bc3(gs, 64, 8), op=ALU.mult))
                    rop(lambda e: e.reciprocal(out=rg16[:], in_=g16[:]))
                    rop(lambda e: e.tensor_tensor(out=RT[:], in0=RE[:], in1=RM[:], op=ALU.mult))
                    tr.op("dve", lambda e: e.tensor_tensor(out=v3(Wt, 16, 32), in0=v3(RT, 16, 32), in1=bc3(rg16, 16, 32),
                                                           op=ALU.mult), reads=[rb], writes=[Wt_b])

                    maskb = sb(pm, "maskb", [128, 512], BF16)
                    cntf = sb(pm, "cntf", [128, NE]); nbf = sb(pm, "nbf", [128, NE]); nbi = sb(pm, "nbi", [128, NE], I32)
                    f16a = sb(pm, "f16a", [128, 16]); f16b = sb(pm, "f16b", [128, 16])
                    rop(lambda e: e.tensor_copy(out=maskb[:], in_=RM[:]))
                    r2, r2b = nb()
                    em = []
                    for tt in range(NT):
                        for j in range(tt):
                            em.append(lambda e, tt=tt, j=j: e.matmul(r2[:, tt * 32:(tt + 1) * 32], lhsT=ones128[:],
                                                                     rhs=maskb[:, j * 32:(j + 1) * 32],
                                                                     start=(j == 0), stop=False))
                        em.append(lambda e, tt=tt: e.matmul(r2[:, tt * 32:(tt + 1) * 32], lhsT=ltri[:],
                                                            rhs=maskb[:, tt * 32:(tt + 1) * 32],
                                                            start=(tt == 0), stop=True))
                    tr.op("pe", em, reads=[rb, cst], writes=[r2b])
                    r3, r3b = nb()
                    em = [lambda e, j=j: e.matmul(r3[:, 0:32], lhsT=ones128[:], rhs=maskb[:, j * 32:(j + 1) * 32],
                                                  start=(j == 0), stop=(j == NT - 1)) for j in range(NT)]
                    tr.op("pe", em, reads=[rb, cst], writes=[r3b])
                    tr.op("dve", lambda e: e.tensor_tensor(out=RT[:], in0=r2[:, :], in1=ecol[:], op=ALU.add),
                          reads=[r2b, cst, rb], writes=[rb])
                    tr.op("dve", lambda e: e.tensor_scalar(out=RL[:], in0=r2[:, :], scalar1=float(CAP), scalar2=None,
                                                           op0=ALU.is_lt), reads=[r2b, rb], writes=[rb])
                    rop(lambda e: e.tensor_tensor(out=RE[:], in0=RM[:], in1=RL[:], op=ALU.mult))
                    rop(lambda e: e.tensor_tensor(out=RT[:], in0=RT[:], in1=RE[:], op=ALU.mult))
                    tr.op("dve", lambda e: e.tensor_tensor(out=Wt[:], in0=Wt[:], in1=RL[:], op=ALU.mult),
                          reads=[rb, Wt_b], writes=[Wt_b])
                    rop(lambda e: e.tensor_reduce(out=r16[:, :], in_=v3(RT, 16, 32), axis=AX.X, op=ALU.max))
                    rop(lambda e: e.tensor_reduce(out=g16[:, :], in_=v3(RT, 16, 32), axis=AX.X, op=ALU.add))
                    f16c = sb(pm, "f16c", [128, 16])
                    rop(lambda e: e.tensor_scalar(out=f16c[:], in0=r16[:], scalar1=0.5, scalar2=None, op0=ALU.is_lt))
                    rop(lambda e: e.tensor_scalar(out=f16a[:], in0=r16[:], scalar1=-1.0, scalar2=None, op0=ALU.add))
                    rop(lambda e: e.scalar_tensor_tensor(out=f16a[:], in0=f16c[:], scalar=float(NROWS + 1), in1=f16a[:],
                                                         op0=ALU.mult, op1=ALU.add))
                    rop(lambda e: e.tensor_tensor(out=f16b[:], in0=g16[:], in1=r16[:], op=ALU.subtract))
                    rop(lambda e: e.tensor_scalar(out=f16c[:], in0=f16b[:], scalar1=0.5, scalar2=None, op0=ALU.is_lt))
                    rop(lambda e: e.tensor_scalar(out=f16b[:], in0=f16b[:], scalar1=-1.0, scalar2=None, op0=ALU.add))
                    rop(lambda e: e.scalar_tensor_tensor(out=f16b[:], in0=f16c[:], scalar=float(NROWS + 1), in1=f16b[:],
                                                         op0=ALU.mult, op1=ALU.add))
                    tr.op("dve", lambda e: e.tensor_copy(out=idx_hi[:], in_=f16a[:]), reads=[rb], writes=[idx_b])
                    tr.op("dve", lambda e: e.tensor_copy(out=idx_lo[:], in_=f16b[:]), reads=[rb], writes=[idx_b])
                    rop(lambda e: e.tensor_tensor(out=v3(RL, 16, 32), in0=v3(RT, 16, 32), in1=bc3(r16, 16, 32),
                                                  op=ALU.is_equal))
                    tr.op("dve", lambda e: e.tensor_tensor(out=RL[:], in0=RL[:], in1=Wt[:], op=ALU.mult),
                          reads=[rb, Wt_b], writes=[rb])
                    tr.op("dve", lambda e: e.tensor_reduce(out=whi[:, :], in_=v3(RL, 16, 32), axis=AX.X, op=ALU.add),
                          reads=[rb], writes=[wgt_b])
                    tr.op("dve", lambda e: e.tensor_reduce(out=f16a[:, :], in_=v3(Wt, 16, 32), axis=AX.X, op=ALU.add),
                          reads=[Wt_b, rb], writes=[rb])
                    tr.op("dve", lambda e: e.tensor_tensor(out=wlo[:], in0=f16a[:], in1=whi[:], op=ALU.subtract),
                          reads=[rb, wgt_b], writes=[wgt_b])
                    tr.op("dve", lambda e: e.tensor_copy(out=cntf[:], in_=r3[:, 0:32]), reads=[r3b, rb], writes=[rb])
                    rop(lambda e: e.tensor_scalar(out=nbf[:], in0=cntf[:], scalar1=0.0, scalar2=None, op0=ALU.is_gt))
                    for j in range(1, JB):
                        rop(lambda e, j=j: e.scalar_tensor_tensor(out=nbf[:], in0=cntf[:], scalar=float(128 * j),
                                                                  in1=nbf[:], op0=ALU.is_gt, op1=ALU.add))
                    rop(lambda e: e.tensor_copy(out=nbi[:], in_=nbf[:]))
                    nblk_b = Buf("nblk")
                    tr.dma("sp", nblk_d, nbi[0:1, :], reads=[rb], writes=[nblk_b])

                    hs = [sb(pm, "hs%d" % i, [128, D], BF16) for i in range(2)]; hs_b = [Buf("hs0"), Buf("hs1")]
                    xgw = [Buf("xgw%d" % i) for i in range(2 * NT)]
                    for tt in range(NT):
                        i = tt % 2
                        tr.dma("sp", hs[i][:], h2d[tt * 128:(tt + 1) * 128, :], reads=[h2d_b[tt]], writes=[hs_b[i]])
                        tr.idma(xg[:, :], idx_hi[:, tt:tt + 1], hs[i][:], None, None,
                                reads=[hs_b[i], idx_b], writes=[xgw[2 * tt]])
                        tr.idma(xg[:, :], idx_lo[:, tt:tt + 1], hs[i][:], None, None,
                                reads=[hs_b[i], idx_b], writes=[xgw[2 * tt + 1]])

                    xb = [sb(pm, "xb%d" % i, [128, D], BF16) for i in range(2)]; xb_b = [Buf("xb0"), Buf("xb1")]
                    xT = [sb(pm, "xT%d" % i, [128, 8, 128], BF16) for i in range(2)]; xT_b = [Buf("xT0"), Buf("xT1")]
                    ab = [sb(pm, "ab%d" % i, [128, DFF], F32) for i in range(2)]; ab_b = [Buf("ab0"), Buf("ab1")]
                    aT = [sb(pm, "aT%d" % i, [128, 4, 128], F32) for i in range(2)]; aT_b = [Buf("aT0"), Buf("aT1")]
                    yb = [sb(pm, "yb%d" % i, [128, D]) for i in range(2)]; yb_b = [Buf("yb0"), Buf("yb1")]
                    blocks = [(ex, j) for ex in range(NE) for j in range(JB)]

                    def prologue(ex):
                        if ex >= 2:
                            wprefetch(ex)
                        for en in ("pe", "act", "dve", "pool", "sp"):
                            tr.wait(en, nblk_b.w)
                        for r in cregs[ex % 3]:
                            nc.reg_load(r, nblk_d[0:1, ex:ex + 1])

                    def e_s1(b):
                        ex, j = blocks[b]
                        c = b % 2
                        if j == 0:
                            prologue(ex)

                        def body():
                            r0 = ex * CAP + j * 128
                            tr.dma("sp", xb[c][:], xg[r0:r0 + 128, :], reads=xgw, writes=[xb_b[c]])
                            bt, bb = nb()
                            btb = bt.bitcast(BF16)
                            em = [lambda e, k=k: e.transpose(btb[:, k * 128:(k + 1) * 128], xb[c][:, k:1024:8],
                                                             ident[:]) for k in range(8)]
                            tr.op("pe", em, reads=[xb_b[c], cst], writes=[bb])
                            tr.op("act", lambda e: e.activation(out=xT[c][:, :, :],
                                                                in_=btb[:, 0:1024].rearrange("p (k t) -> p k t", k=8),
                                                                func=AF.Identity), reads=[bb], writes=[xT_b[c]])
                        tr.cond_block(cregs[ex % 3], j, body)

                    def e_s2(b):
                        ex, j = blocks[b]
                        c = b % 2
                        i = ex % 2

                        def body():
                            bg, bgb = nb()
                            em = [lambda e, k=k: e.matmul(bg[:, :], lhsT=xT[c][:, k, :], rhs=wg[i][:, k, :],
                                                          start=(k == 0), stop=(k == 7)) for k in range(8)]
                            tr.op("pe", em, reads=[xT_b[c], wg_b[i]], writes=[bgb])
                            bu, bub = nb()
                            em = [lambda e, k=k: e.matmul(bu[:, :], lhsT=xT[c][:, k, :], rhs=wu[i][:, k, :],
                                                          start=(k == 0), stop=(k == 7)) for k in range(8)]
                            tr.op("pe", em, reads=[xT_b[c], wu_b[i]], writes=[bub])
                            tr.op("act", lambda e: e.activation(out=sl[c][:], in_=bg[:, :], func=AF.Silu),
                                  reads=[bgb], writes=[sl_b[c]])
                            tr.op("dve", lambda e: e.tensor_tensor(out=ab[c][:], in0=sl[c][:], in1=bu[:, :], op=ALU.mult),
                                  reads=[sl_b[c], bub], writes=[ab_b[c]])
                        tr.cond_block(cregs[ex % 3], j, body)

                    def e_s3(b):
                        ex, j = blocks[b]
                        c = b % 2

                        def body():
                            b2, b2b = nb()
                            em = [lambda e, k=k: e.transpose(b2[:, k * 128:(k + 1) * 128], ab[c][:, k:512:4],
                                                             identf[:]) for k in range(4)]
                            tr.op("pe", em, reads=[ab_b[c], cst], writes=[b2b])
                            tr.op("act", lambda e: e.activation(out=aT[c][:, :, :],
                                                                in_=b2[:, 0:512].rearrange("p (k t) -> p k t", k=4),
                                                                func=AF.Identity), reads=[b2b], writes=[aT_b[c]])
                        tr.cond_block(cregs[ex % 3], j, body)

                    def e_s4(b):
                        ex, j = blocks[b]
                        c = b % 2
                        i = ex % 2

                        def body():
                            r0 = ex * CAP + j * 128
                            for half in range(2):
                                bo, bob = nb()
                                em = [lambda e, k=k, bo=bo: e.matmul(bo[:, :], lhsT=aT[c][:, k, :],
                                                                     rhs=wd[i][:, k, half * 512:(half + 1) * 512],
                                                                     start=(k == 0), stop=(k == 3)) for k in range(4)]
                                tr.op("pe", em, reads=[aT_b[c], wd_b[i]], writes=[bob])
                                if half == 0:
                                    tr.op("act", lambda e, bo=bo: e.activation(out=yb[c][:, 0:512], in_=bo[:, :],
                                                                               func=AF.Identity),
                                          reads=[bob], writes=[yb_b[c]])
                                else:
                                    tr.op("dve", lambda e, bo=bo: e.tensor_copy(out=yb[c][:, 512:1024], in_=bo[:, :]),
                                          reads=[bob], writes=[yb_b[c]])
                            tr.dma("sp", yg[r0:r0 + 128, :], yb[c][:], reads=[yb_b[c]], writes=[yg_b[ex]])
                        tr.cond_block(cregs[ex % 3], j, body)
                    pipeline(len(blocks), [e_s1, e_s2, e_s3, e_s4], "EX")
                    tr.barrier()

                with ExitStack() as p4:
                    def mk(nm, n):
                        return [sb(p4, "%s%d" % (nm, i), [128, D]) for i in range(n)], [Buf("%s%d" % (nm, i)) for i in range(n)]
                    xt, xt_b = mk("xt", 4); yh, yh_b = mk("yh", 2); yl, yl_b = mk("yl", 3)
                    t1, t1_b = mk("t1", 2); ua, ua_b = mk("ua", 2); t2, t2_b = mk("t2", 2)
                    uu, uu_b = mk("uu", 4); xn, xn_b = mk("xn", 2); ub, ub_b = mk("ub", 2); xo, xo_b = mk("xo", 2)
                    lnt = ln_bufs(p4, 4)
                    rsn = {}

                    def l_s0(tt):
                        tr.dma("sp", xt[tt % 4][:], xs[tt * 128:(tt + 1) * 128, :], writes=[xt_b[tt % 4]])
                        tr.idma(yh[tt % 2][:], None, yg[:, :], idx_hi[:, tt:tt + 1], None,
                                reads=yg_b + [idx_b], writes=[yh_b[tt % 2]])
                        tr.idma(yl[tt % 3][:], None, yg[:, :], idx_lo[:, tt:tt + 1], None,
                                reads=yg_b + [idx_b], writes=[yl_b[tt % 3]])

                    def l_s1(tt):
                        i = tt % 2
                        tr.op("act", lambda e: e.activation(out=t1[i][:], in_=yh[i][:], func=AF.Identity,
                                                            scale=whi[:, tt:tt + 1]),
                              reads=[yh_b[i], wgt_b], writes=[t1_b[i]])

                    def l_s2(tt):
                        i = tt % 2
                        tr.op("dve", lambda e: e.scalar_tensor_tensor(out=ua[i][:], in0=yl[tt % 3][:],
                                                                      scalar=wlo[:, tt:tt + 1], in1=t1[i][:],
                                                                      op0=ALU.mult, op1=ALU.add),
                              reads=[yl_b[tt % 3], t1_b[i], wgt_b], writes=[ua_b[i]])
                        tr.op("pool" if tt % 3 == 2 else "dve",
                              lambda e: e.tensor_tensor(out=t2[i][:], in0=ua[i][:], in1=G2[:], op=ALU.mult),
                              reads=[ua_b[i], bG2], writes=[t2_b[i]])

                    def l_s3(tt):
                        i = tt % 2
                        i4 = tt % 4
                        tr.op("dve", lambda e: e.scalar_tensor_tensor(out=uu[i4][:], in0=xt[i4][:], scalar=ALPHA,
                                                                      in1=t2[i][:], op0=ALU.mult, op1=ALU.add),
                              reads=[xt_b[i4], t2_b[i]], writes=[uu_b[i4]])
                        ln_stats(lnt[i4], uu[i4], uu_b[i4], D, 2)

                    def l_s4(tt):
                        ln_sqrt(lnt[tt % 4])

                    def l_s5(tt):
                        rsn[tt] = ln_rs(lnt[tt % 4])

                    def l_s6(tt):
                        i = tt % 2
                        i4 = tt % 4
                        rs, nmr, sb_ = rsn.pop(tt)
                        tr.op("act", lambda e: e.activation(out=xn[i][:], in_=uu[i4][:], func=AF.Identity,
                                                            scale=rs[:, 0:1], bias=nmr[:, 0:1]),
                              reads=[uu_b[i4]] + sb_, writes=[xn_b[i]])

                    def l_s7(tt):
                        i = tt % 2
                        tr.op("dve", lambda e: e.tensor_tensor(out=ub[i][:], in0=xn[i][:], in1=LG[:], op=ALU.mult),
                              reads=[xn_b[i], bLG], writes=[ub_b[i]])
                        tr.op("pool" if tt % 3 == 0 else "dve",
                              lambda e: e.tensor_tensor(out=xo[i][:], in0=ub[i][:], in1=LB[:], op=ALU.add),
                              reads=[ub_b[i], bLB], writes=[xo_b[i]])
                        tr.dma("pool", x_dst[tt * 128:(tt + 1) * 128, :], xo[i][:], reads=[xo_b[i]])
                    pipeline(NT, [l_s0, l_s1, l_s2, l_s3, l_s4, l_s5, l_s6, l_s7], "LN")
                    tr.barrier()
            x_src = xs
        tr.barrier(engines=["sp"])
    return nc


def _prep_inputs(inp):
    f = np.float32
    g = lambda k: np.ascontiguousarray(np.asarray(inp[k], dtype=f))
    conv_w = g("conv_w"); conv_b = g("conv_b")
    cw = np.zeros((128, DEPTH, 3, 4), f)
    for l in range(DEPTH):
        for j in range(3):
            for k in range(3):
                cw[:, l, j, k] = conv_w[l, k, j * 128:(j + 1) * 128]
            cw[:, l, j, 3] = conv_b[l, j * 128:(j + 1) * 128]
    psc = np.ascontiguousarray(g("pool_scale").reshape(DEPTH, 2, 128).transpose(2, 0, 1).reshape(128, DEPTH * 2))
    t = np.arange(128)
    cmask = (t[:, None] <= t[None, :]).astype(f)
    wins = (2, 4, 8, 16)
    invw = np.zeros((128, 2), f); ic16 = np.zeros((128, 2, 16), f)
    for c in range(2):
        for hh in range(2):
            w = wins[c * 2 + hh]
            invw[hh * 64:(hh + 1) * 64, c] = 1.0 / w
            ic16[hh * 64:(hh + 1) * 64, c, :] = 1.0 / np.minimum(np.arange(16) + 1, w)
    shared = {
        "w_ada": g("w_ada"), "b_ada": g("b_ada"), "w_in": g("w_in"),
        "cw": cw.reshape(128, -1), "pool_w": g("pool_w"), "psc": psc,
        "sgu_ln_g": g("sgu_ln_g"), "sgu_ln_b": g("sgu_ln_b"),
        "sgu_wT": np.ascontiguousarray(g("sgu_w").transpose(0, 1, 3, 2)),
        "sgu_b": g("sgu_b").reshape(DEPTH, 6 * 128), "w_out": g("w_out"),
        "ln1_g": g("ln1_g"), "ln1_b": g("ln1_b"), "w_router": g("w_router"),
        "b_router": g("b_router").reshape(1, NE), "w_gate": g("w_gate"), "w_up": g("w_up"),
        "w_down": g("w_down"), "ln2_g": g("ln2_g"), "ln2_b": g("ln2_b"),
        "ident": np.eye(128, dtype=f), "cmask": cmask, "invw": invw, "ic16": ic16.reshape(128, 32),
        "ltri": (t[:, None] < t[None, :]).astype(f),
        "ecol": np.ascontiguousarray(np.broadcast_to(np.tile(np.arange(NE, dtype=f) * CAP + 1.0, NT)[None, :], (128, NT * NE))),
    }
    x = g("x"); c = g("c")
    maps = []
    for b in range(8):
        m = dict(shared)
        m["x"] = x[b]
        m["cT"] = np.ascontiguousarray(c[b].reshape(8, 128).T)
        maps.append(m)
    return maps


_NC_CACHE = {}


def kernel(**inputs):
    n_layers = DEPTH
    if n_layers not in _NC_CACHE:
        _NC_CACHE[n_layers] = build(n_layers)
    nc = _NC_CACHE[n_layers]
    maps = _prep_inputs(inputs)
    res = run_bass_kernel_spmd(nc, maps, core_ids=list(range(8)))
    return np.stack([np.asarray(r["y"], dtype=np.float32) for r in res.results], axis=0)
```

```python
import numpy as np
from contextlib import ExitStack
import concourse.bass as bass
import concourse.mybir as mybir
from concourse.bass_utils import run_bass_kernel_spmd

F32 = mybir.dt.float32
BF16 = mybir.dt.bfloat16
ALU = mybir.AluOpType
AF = mybir.ActivationFunctionType
AX = mybir.AxisListType

D = 1024
S = 2048
NT = S // 128
DEPTH = 4
D_IN = 2176
NE = 32
DFF = 512
JB = 8
CAP = JB * 128
NROWS = NE * CAP
I32 = mybir.dt.int32
ET = mybir.EngineType
ALPHA = float((2 * DEPTH) ** 0.25)
EPS = 1e-5


class Buf:
    def __init__(self, name, excl=False):
        self.name = name
        self.w = None
        self.r = {}
        self.excl = excl


class TR:
    def __init__(self, nc, es, ndma_sp=8, ndma_pool=8):
        self.nc = nc
        self.eng = {"pe": nc.tensor, "act": nc.scalar, "dve": nc.vector, "pool": nc.gpsimd, "sp": nc.sync}
        self.sem = {}
        self.cnt = {}
        for k in self.eng:
            self.sem[k] = es.enter_context(nc.semaphore("s_" + k))
            self.cnt[k] = 0
        self.dq = {"sp": [], "pool": []}
        for q, n in (("sp", ndma_sp), ("pool", ndma_pool)):
            for i in range(n):
                key = "d_%s%d" % (q, i)
                self.sem[key] = es.enter_context(nc.semaphore(key))
                self.cnt[key] = 0
                self.dq[q].append(key)
        self.dnext = {"sp": 0, "pool": 0}
        self.seen = {k: {} for k in self.eng}

    def wait(self, eng, dep):
        key, val = dep
        if key == "pe" and eng == "pe":
            return
        if self.seen[eng].get(key, 0) >= val:
            return
        self.eng[eng].wait_ge(self.sem[key], val)
        self.seen[eng][key] = val

    def _deps(self, reads, writes):
        deps = []
        for b in reads:
            if b.w is not None:
                deps.append(b.w)
            if b.excl:
                deps += list(b.r.items())
        for b in writes:
            if b.w is not None:
                deps.append(b.w)
            deps += list(b.r.items())
        best = {}
        for k, v in deps:
            if v > best.get(k, 0):
                best[k] = v
        return list(best.items())

    def _reg(self, tok, reads, writes):
        for b in reads:
            if b.excl:
                b.w = tok
                b.r = {}
            else:
                b.r[tok[0]] = max(b.r.get(tok[0], 0), tok[1])
        for b in writes:
            b.w = tok
            b.r = {}

    def op(self, eng, emits, reads=(), writes=()):
        for d in self._deps(reads, writes):
            self.wait(eng, d)
        if not isinstance(emits, (list, tuple)):
            emits = [emits]
        ins = None
        for f in emits:
            ins = f(self.eng[eng])
        self.cnt[eng] += 1
        ins.then_inc(self.sem[eng], 1)
        tok = (eng, self.cnt[eng])
        self._reg(tok, reads, writes)
        return tok

    def dma(self, q, out, in_, reads=(), writes=(), **kw):
        key = self.dq[q][self.dnext[q]]
        self.dnext[q] = (self.dnext[q] + 1) % len(self.dq[q])
        if self.cnt[key] > 0:
            self.wait(q, (key, 16 * self.cnt[key]))
        for d in self._deps(reads, writes):
            self.wait(q, d)
        ins = self.eng[q].dma_start(out=out, in_=in_, **kw)
        self.cnt[key] += 1
        ins.then_inc(self.sem[key], 16)
        tok = (key, 16 * self.cnt[key])
        self._reg(tok, reads, writes)
        return tok

    def idma(self, out, out_off, in_, in_off, bounds, reads=(), writes=()):
        q = "pool"
        key = self.dq[q][self.dnext[q]]
        self.dnext[q] = (self.dnext[q] + 1) % len(self.dq[q])
        if self.cnt[key] > 0:
            self.wait(q, (key, 16 * self.cnt[key]))
        for d in self._deps(reads, writes):
            self.wait(q, d)
        ins = self.nc.gpsimd.indirect_dma_start(
            out=out, out_offset=(bass.IndirectOffsetOnAxis(ap=out_off, axis=0) if out_off is not None else None),
            in_=in_, in_offset=(bass.IndirectOffsetOnAxis(ap=in_off, axis=0) if in_off is not None else None),
            bounds_check=bounds, oob_is_err=False)
        self.cnt[key] += 1
        ins.then_inc(self.sem[key], 16)
        tok = (key, 16 * self.cnt[key])
        self._reg(tok, reads, writes)
        return tok

    def cond_block(self, regs, j, body):
        before = dict(self.cnt)
        seen0 = {e: dict(d) for e, d in self.seen.items()}
        with self.nc.If_cmp(regs, j, "IS_GT"):
            body()
        after = dict(self.cnt)
        with self.nc.Else():
            for key in after:
                d = after[key] - before[key]
                if d == 0:
                    continue
                if key in self.eng:
                    owner, unit = key, 1
                else:
                    owner, unit = ("sp" if key.startswith("d_sp") else "pool"), 16
                if before[key] > 0:
                    self.eng[owner].wait_ge(self.sem[key], unit * before[key])
                self.eng[owner].sem_inc(self.sem[key], unit * d)
        self.seen = seen0

    def all_tokens(self):
        toks = []
        for k, c in self.cnt.items():
            if c > 0:
                toks.append((k, c if k in self.eng else 16 * c))
        return toks

    def barrier(self, engines=None):
        toks = self.all_tokens()
        for e in (engines or self.eng):
            for t in toks:
                if t[0] == e and e in ("pe",):
                    continue
                self.wait(e, t)


def build(n_layers=DEPTH):
    nc = bass.Bass("TRN2", target_bir_lowering=False)

    def din(name, shape, dt=F32):
        return nc.dram_tensor(name, list(shape), dt, kind="ExternalInput").ap()

    x_in = din("x", [S, D])
    cT_in = din("cT", [128, 8])
    w_ada = din("w_ada", [DEPTH, D, 6 * D])
    b_ada = din("b_ada", [DEPTH, 6 * D])
    w_in = din("w_in", [DEPTH, D, D_IN])
    cw_in = din("cw", [128, DEPTH * 3 * 4])
    pool_w = din("pool_w", [DEPTH, 4, 64, 64])
    psc_in = din("psc", [128, DEPTH * 2])
    sgu_ln_g = din("sgu_ln_g", [DEPTH, 384])
    sgu_ln_b = din("sgu_ln_b", [DEPTH, 384])
    sgu_wT = din("sgu_wT", [DEPTH, 6, 128, 128])
    sgu_b = din("sgu_b", [DEPTH, 6 * 128])
    w_out = din("w_out", [DEPTH, D, D])
    ln1_g = din("ln1_g", [DEPTH, D])
    ln1_b = din("ln1_b", [DEPTH, D])
    w_router = din("w_router", [D, NE])
    b_router = din("b_router", [1, NE])
    w_gate = din("w_gate", [DEPTH, NE, D, DFF])
    w_up = din("w_up", [DEPTH, NE, D, DFF])
    w_down = din("w_down", [DEPTH, NE, DFF, D])
    ln2_g = din("ln2_g", [DEPTH, D])
    ln2_b = din("ln2_b", [DEPTH, D])
    ident_in = din("ident", [128, 128])
    cmask_in = din("cmask", [128, 128])
    invw_in = din("invw", [128, 2])
    ic16_in = din("ic16", [128, 32])
    ltri_in = din("ltri", [128, 128])
    ecol_in = din("ecol", [128, 512])
    y_out = nc.dram_tensor("y", [S, D], F32, kind="ExternalOutput").ap()
    xs = nc.dram_tensor("xs", [S, D], F32, kind="Internal").ap()
    h2d = nc.dram_tensor("h2d", [S, D], BF16, kind="Internal").ap()
    xg = nc.dram_tensor("xg", [NROWS + 1, D], BF16, kind="Internal").ap()
    yg = nc.dram_tensor("yg", [NROWS + 1, D], F32, kind="Internal").ap()
    nblk_d = nc.dram_tensor("nblk_d", [1, NE], I32, kind="Internal").ap()

    es = ExitStack()
    with es:
        tr = TR(nc, es)

        uid = [0]

        def sb(scope, name, shape, dt=F32):
            uid[0] += 1
            return scope.enter_context(nc.sbuf_tensor("%s_u%d" % (name, uid[0]), list(shape), dt))

        banks = []
        for i in range(8):
            t = es.enter_context(nc.psum_tensor("ps%d" % i, [128, 512], F32))
            banks.append((t, Buf("ps%d" % i, excl=True)))
        bank_i = [0]

        def nb():
            b = banks[bank_i[0]]
            bank_i[0] = (bank_i[0] + 1) % 8
            return b

        hT = sb(es, "hT", [128, 8, S], BF16)
        hT_b = [Buf("hT%d" % i) for i in range(NT)]
        ident = sb(es, "ident", [128, 128], BF16)
        cmask = sb(es, "cmask", [128, 128], BF16)
        ones_f = sb(es, "ones_f", [1, 128], F32)
        ones_b = sb(es, "ones_b", [1, 128], BF16)
        cb = sb(es, "cb", [128, 8, 128], F32)
        cbb = sb(es, "cbb", [128, 8, 128], BF16)
        cw = sb(es, "cw", [128, DEPTH * 12], F32)
        psc = sb(es, "psc", [128, DEPTH * 2], F32)
        invw = sb(es, "invw", [128, 2], F32)
        ic16 = sb(es, "ic16", [128, 32], F32)
        wr = sb(es, "wr", [128, 8, NE], BF16)
        brr = sb(es, "brr", [1, NE], BF16)
        cst = Buf("consts")
        ltri = sb(es, "ltri", [128, 128], BF16)
        ones128 = sb(es, "ones128", [128, 128], BF16)
        ecol = sb(es, "ecol", [128, 512], F32)
        h2d_b = [Buf("h2d%d" % i) for i in range(NT)]
        cregs = [nc.alloc_registers("nblk%d" % q, [ET.PE, ET.Activation, ET.DVE, ET.Pool, ET.SP]) for q in range(3)]

        with ExitStack() as s0:
            cT = sb(s0, "cT", [128, 8], F32)
            cact = sb(s0, "cact", [128, 8], F32)
            b_cT = Buf("cT")
            b_cact = Buf("cact")
            tr.dma("sp", cT[:], cT_in, writes=[b_cT])
            tr.dma("sp", cw[:], cw_in, writes=[cst])
            tr.dma("sp", psc[:], psc_in, writes=[cst])
            tr.dma("sp", invw[:], invw_in, writes=[cst])
            tr.dma("sp", ic16[:], ic16_in, writes=[cst])
            tr.dma("pool", ident[:], ident_in, writes=[cst])
            tr.dma("pool", cmask[:], cmask_in, writes=[cst])
            tr.dma("pool", wr[:], w_router.rearrange("(k p) n -> p k n", p=128), writes=[cst])
            tr.dma("pool", brr[:], b_router, writes=[cst])
            tr.dma("pool", ltri[:], ltri_in, writes=[cst])
            tr.dma("sp", ecol[:], ecol_in, writes=[cst])
            tr.op("dve", lambda e: e.memset(ones128[:], 1.0), writes=[cst])
            zrow = sb(s0, "zrow", [1, D], F32)
            b_z = Buf("zrow")
            tr.op("dve", lambda e: e.memset(zrow[:], 0.0), writes=[b_z])
            tr.dma("sp", yg[NROWS:NROWS + 1, :], zrow[:], reads=[b_z])
            tr.op("dve", lambda e: e.memset(ones_f[:], 1.0), writes=[cst])
            tr.op("dve", lambda e: e.memset(ones_b[:], 1.0), writes=[cst])
            tr.op("act", lambda e: e.activation(out=cact[:], in_=cT[:], func=AF.Silu), reads=[b_cT], writes=[b_cact])
            for k in range(8):
                tr.op("dve", lambda e, k=k: e.tensor_copy(out=cb[:, k, :], in_=cact[:, k:k + 1].to_broadcast([128, 128])),
                      reads=[b_cact], writes=[cst])
            tr.op("dve", lambda e: e.tensor_copy(out=cbb[:], in_=cb[:]), reads=[cst], writes=[cst])
            tr.barrier()

        def ada_chunk(scope_bufs, l, j, dst, dst_b, add_one):
            wa, wa_b, brow, brow_b, wab, wab_b = scope_bufs
            for half in range(2):
                c0 = j * 1024 + half * 512
                i = (j * 2 + half) % 2
                tr.dma("sp", wa[i][:], w_ada[l].rearrange("(k p) n -> p k n", p=128)[:, :, c0:c0 + 512],
                       writes=[wa_b[i]])
                tr.op("dve", lambda e, i=i: e.tensor_copy(out=wab[i][:], in_=wa[i][:]), reads=[wa_b[i]], writes=[wab_b[i]])
                bt, bb = nb()
                em = []
                for k in range(8):
                    em.append(lambda e, k=k, i=i, bt=bt: e.matmul(bt[:, :], lhsT=cbb[:, k, :], rhs=wab[i][:, k, :],
                                                                  start=(k == 0), stop=False))
                em.append(lambda e, bt=bt, c0=c0: e.matmul(bt[:, :], lhsT=ones_f[0:1, :], rhs=brow[0:1, c0:c0 + 512],
                                                           start=False, stop=True))
                tr.op("pe", em, reads=[wab_b[i], brow_b, cst], writes=[bb])
                tr.op("act", lambda e, bt=bt, half=half: e.activation(
                    out=dst[:, half * 512:(half + 1) * 512], in_=bt[:, :], func=AF.Identity,
                    bias=(1.0 if add_one else 0.0)), reads=[bb], writes=[dst_b])

        def bcast_load(dst, dst_b, src_row):
            tr.dma("sp", dst[:], src_row.partition_broadcast(128), writes=[dst_b])

        def ln_stats(scope_t, u, u_b, width, nchunk):
            st, mv, rs, nmr, b_st = scope_t
            em = []
            cwid = width // nchunk
            for c in range(nchunk):
                em.append(lambda e, c=c: e.bn_stats(out=st[:, c, :], in_=u[:, c * cwid:(c + 1) * cwid]))
            tr.op("dve", em, reads=[u_b], writes=[b_st[0]])
            tr.op("dve", lambda e: e.bn_aggr(out=mv[:, :], in_=st[:, 0:nchunk, :]), reads=[b_st[0]], writes=[b_st[1]])

        def ln_rstd(scope_t):
            st, mv, rs, nmr, b_st = scope_t
            tr.op("act", lambda e: e.activation(out=nmr[:, :], in_=mv[:, 1:2], func=AF.Sqrt, bias=EPS),
                  reads=[b_st[1]], writes=[b_st[3]])
            tr.op("dve", lambda e: e.reciprocal(out=rs[:, :], in_=nmr[:, :]), reads=[b_st[3]], writes=[b_st[2]])
            tr.op("dve", lambda e: e.scalar_tensor_tensor(out=nmr[:, :], in0=mv[:, 0:1], scalar=-1.0, in1=rs[:, :],
                                                          op0=ALU.mult, op1=ALU.mult),
                  reads=[b_st[1], b_st[2]], writes=[b_st[3]])
            return rs, nmr, [b_st[2], b_st[3]]

        def ln_sqrt(scope_t):
            st, mv, rs, nmr, b_st = scope_t
            tr.op("act", lambda e: e.activation(out=nmr[:, :], in_=mv[:, 1:2], func=AF.Sqrt, bias=EPS),
                  reads=[b_st[1]], writes=[b_st[3]])

        def ln_rs(scope_t):
            st, mv, rs, nmr, b_st = scope_t
            tr.op("dve", lambda e: e.reciprocal(out=rs[:, :], in_=nmr[:, :]), reads=[b_st[3]], writes=[b_st[2]])
            tr.op("dve", lambda e: e.scalar_tensor_tensor(out=nmr[:, :], in0=mv[:, 0:1], scalar=-1.0, in1=rs[:, :],
                                                          op0=ALU.mult, op1=ALU.mult),
                  reads=[b_st[1], b_st[2]], writes=[b_st[3]])
            return rs, nmr, [b_st[2], b_st[3]]

        def ln_tile(scope_t, u, u_b, width, nchunk):
            ln_stats(scope_t, u, u_b, width, nchunk)
            return ln_rstd(scope_t)

        def ln_bufs(scope, n):
            out = []
            for i in range(n):
                out.append((sb(scope, "st%d" % i, [128, 2, 6]), sb(scope, "mv%d" % i, [128, 2]),
                            sb(scope, "rs%d" % i, [128, 1]), sb(scope, "nmr%d" % i, [128, 1]),
                            [Buf("lnst%d_%d" % (i, q)) for q in range(4)]))
            return out

        def pipeline(n, stages, name=""):
            import os
            if name and name in os.environ.get("PIPE_OFF", ""):
                for it in range(n):
                    for f in stages:
                        f(it)
                return
            for step in range(n + len(stages) - 1):
                for si, f in enumerate(stages):
                    it = step - si
                    if 0 <= it < n:
                        f(it)

        def transpose_to_hT(src, src_b, tt):
            bt, bb = nb()
            btb = bt.bitcast(BF16)
            em = []
            for k in range(8):
                em.append(lambda e, k=k: e.transpose(btb[:, k * 128:(k + 1) * 128], src[:, k * 128:(k + 1) * 128], ident[:]))
            tr.op("pe", em, reads=[src_b, cst], writes=[bb])
            tr.op("act", lambda e: e.activation(out=hT[:, :, tt * 128:(tt + 1) * 128],
                                                in_=btb[:, 0:1024].rearrange("p (k t) -> p k t", k=8),
                                                func=AF.Identity), reads=[bb], writes=[hT_b[tt]])

        x_src = x_in
        for l in range(n_layers):
            last = (l == n_layers - 1)
            x_dst = y_out if last else xs
            with ExitStack() as pa:
                M1 = sb(pa, "M1", [128, D]); S1 = sb(pa, "S1", [128, D]); G1 = sb(pa, "G1", [128, D])
                M2 = sb(pa, "M2", [128, D]); S2 = sb(pa, "S2", [128, D])
                LG = sb(pa, "LG1", [128, D]); LB = sb(pa, "LB1", [128, D])
                bM1, bS1, bG1, bM2, bS2, bLG, bLB = [Buf(n) for n in ("M1", "S1", "G1", "M2", "S2", "LG", "LB")]
                with ExitStack() as pt:
                    wa = [sb(pt, "wa%d" % i, [128, 8, 512]) for i in range(2)]
                    wa_b = [Buf("wa0"), Buf("wa1")]
                    brow = sb(pt, "brow", [1, 6 * D]); brow_b = Buf("brow")
                    tr.dma("sp", brow[:], b_ada[l:l + 1, :], writes=[brow_b])
                    wab = [sb(pt, "wab%d" % i, [128, 8, 512], BF16) for i in range(2)]
                    wab_b = [Buf("wab0"), Buf("wab1")]
                    sc = (wa, wa_b, brow, brow_b, wab, wab_b)
                    ada_chunk(sc, l, 0, S1, bS1, False)
                    ada_chunk(sc, l, 1, M1, bM1, True)
                    ada_chunk(sc, l, 2, G1, bG1, False)
                    ada_chunk(sc, l, 3, S2, bS2, False)
                    ada_chunk(sc, l, 4, M2, bM2, True)
                    bcast_load(LG, bLG, ln1_g[l:l + 1, :])
                    bcast_load(LB, bLB, ln1_b[l:l + 1, :])
                    tr.barrier()

                yT = sb(pa, "yT", [128, 8, S], BF16)
                yT_b = [Buf("yT%d" % i) for i in range(8)]
                xt = [sb(pa, "xt%d" % i, [128, D]) for i in range(3)]
                xt_b = [Buf("xt0"), Buf("xt1"), Buf("xt2")]
                tmp = [sb(pa, "tmp%d" % i, [128, D]) for i in range(2)]
                tmp_b = [Buf("tmp0"), Buf("tmp1")]
                hb = [sb(pa, "hb%d" % i, [128, D], BF16) for i in range(2)]
                hb_b = [Buf("hb0"), Buf("hb1")]

                def a1_ld(tt):
                    tr.dma("sp", xt[tt % 3][:], x_src[tt * 128:(tt + 1) * 128, :], writes=[xt_b[tt % 3]])

                def a1_s0(tt):
                    i = tt % 2
                    tr.op("dve", lambda e: e.tensor_tensor(out=tmp[i][:], in0=xt[tt % 3][:], in1=M1[:], op=ALU.mult),
                          reads=[xt_b[tt % 3], bM1], writes=[tmp_b[i]])
                    tr.op("pool" if tt % 3 == 2 else "dve",
                          lambda e: e.tensor_tensor(out=hb[i][:], in0=tmp[i][:], in1=S1[:], op=ALU.add),
                          reads=[tmp_b[i], bS1], writes=[hb_b[i]])

                def a1_s1(tt):
                    transpose_to_hT(hb[tt % 2], hb_b[tt % 2], tt)
                pipeline(NT, [a1_ld, a1_s0, a1_s1], "A1")

                with ExitStack() as p2:
                    W = [sb(p2, "W%d" % i, [128, S + 16]) for i in range(4)]
                    W_b = [Buf("W%d" % i) for i in range(4)]
                    wc = [sb(p2, "wc%d" % i, [128, 8, 128], BF16) for i in range(4)]
                    wc_b = [Buf("wc%d" % i) for i in range(4)]
                    wci = [0]
                    pbf = sb(p2, "pbf", [128, S], BF16); pbf_b = Buf("pbf")
                    pwbd = sb(p2, "pwbd", [128, 2, 128], BF16); pwbd_b = Buf("pwbd")
                    wv = sb(p2, "wv", [128, 8, 384], BF16); wv_b = Buf("wv")
                    vnb = sb(p2, "vnb", [128, NT, 384], BF16); vnb_b = [Buf("vnb%d" % i) for i in range(NT)]
                    SG = sb(p2, "SG", [128, 384]); SBt = sb(p2, "SBt", [128, 384]); bSG = Buf("SG"); bSB = Buf("SB")
                    wsm = sb(p2, "wsm", [128, 6, 128], BF16); wsm_b = Buf("wsm")
                    wsr = sb(p2, "wsr", [128, 6, 128], BF16); wsr_b = Buf("wsr")
                    bsr = sb(p2, "bsr", [1, 6 * 128], BF16); bsr_b = Buf("bsr")
                    vt = [sb(p2, "vt%d" % i, [128, 384]) for i in range(2)]; vt_b = [Buf("vt0"), Buf("vt1")]
                    vt2 = [sb(p2, "vu%d" % i, [128, 384]) for i in range(2)]; vt2_b = [Buf("vu0"), Buf("vu1")]
                    lnt = ln_bufs(p2, 4)

                    w_in_l = w_in[l].rearrange("(k p) n -> p k n", p=128)

                    def zmm(f):
                        i = wci[0]
                        wci[0] = (wci[0] + 1) % 4
                        tr.dma("pool", wc[i][:], w_in_l[:, :, f * 128:(f + 1) * 128], writes=[wc_b[i]])
                        out = []
                        for tg in range(4):
                            bt, bb = nb()
                            em = [lambda e, k=k, bt=bt, tg=tg: e.matmul(bt[:, :], lhsT=wc[i][:, k, :],
                                                                        rhs=hT[:, k, tg * 512:(tg + 1) * 512],
                                                                        start=(k == 0), stop=(k == 7)) for k in range(8)]
                            tr.op("pe", em, reads=[wc_b[i]] + hT_b[tg * 4:(tg + 1) * 4], writes=[bb])
                            out.append((bt, bb))
                        return out

                    for j in range(3):
                        cwo = (l * 3 + j) * 4
                        tr.op("dve", lambda e: e.memset(W[1][:, 0:2], 0.0), writes=[W_b[1]])
                        bk = zmm(3 + j)
                        for tg in range(4):
                            tr.op("act", lambda e, tg=tg, bt=bk[tg][0]: e.activation(
                                out=W[0][:, tg * 512:(tg + 1) * 512], in_=bt[:, :], func=AF.Identity),
                                reads=[bk[tg][1]], writes=[W_b[0]])
                        bk = zmm(6 + j)
                        for tg in range(4):
                            tr.op("dve", lambda e, tg=tg, bt=bk[tg][0]: e.tensor_tensor(
                                out=W[1][:, 2 + tg * 512:2 + (tg + 1) * 512], in0=W[0][:, tg * 512:(tg + 1) * 512],
                                in1=bt[:, :], op=ALU.mult), reads=[W_b[0], bk[tg][1]], writes=[W_b[1]])
                        tr.op("dve", lambda e: e.tensor_scalar(out=W[2][:, 0:S], in0=W[1][:, 2:2 + S],
                                                               scalar1=cw[:, cwo + 2:cwo + 3], scalar2=cw[:, cwo + 3:cwo + 4],
                                                               op0=ALU.mult, op1=ALU.add),
                              reads=[W_b[1], cst], writes=[W_b[2]])
                        tr.op("dve", lambda e: e.scalar_tensor_tensor(out=W[3][:, 0:S], in0=W[1][:, 1:1 + S],
                                                                      scalar=cw[:, cwo + 1:cwo + 2], in1=W[2][:, 0:S],
                                                                      op0=ALU.mult, op1=ALU.add),
                              reads=[W_b[1], W_b[2], cst], writes=[W_b[3]])
                        tr.op("dve", lambda e: e.scalar_tensor_tensor(out=W[2][:, 0:S], in0=W[1][:, 0:S],
                                                                      scalar=cw[:, cwo:cwo + 1], in1=W[3][:, 0:S],
                                                                      op0=ALU.mult, op1=ALU.add),
                              reads=[W_b[1], W_b[3], cst], writes=[W_b[2]])
                        bk = zmm(j)
                        for tg in range(4):
                            tr.op("dve", lambda e, tg=tg, bt=bk[tg][0]: e.tensor_tensor(
                                out=yT[:, j, tg * 512:(tg + 1) * 512], in0=W[2][:, tg * 512:(tg + 1) * 512],
                                in1=bt[:, :], op=ALU.mult), reads=[W_b[2], bk[tg][1]], writes=[yT_b[j]])

                    tr.op("dve", lambda e: e.memset(pwbd[:], 0.0), writes=[pwbd_b])
                    for g in range(4):
                        o = (g % 2) * 64
                        tr.dma("pool", pwbd[o:o + 64, g // 2, o:o + 64], pool_w[l, g], writes=[pwbd_b])
                    for c in range(2):
                        for i in range(4):
                            tr.op("dve", lambda e, i=i: e.memset(W[i][:, 0:16], 0.0), writes=[W_b[i]])
                        bk = zmm(9 + c)
                        for tg in range(4):
                            tr.op("act", lambda e, tg=tg, bt=bk[tg][0]: e.activation(
                                out=W[0][:, 16 + tg * 512:16 + (tg + 1) * 512], in_=bt[:, :], func=AF.Identity),
                                reads=[bk[tg][1]], writes=[W_b[0]])

                        def lvl(dst, src, sh):
                            tr.op("dve", lambda e: e.tensor_tensor(out=W[dst][:, 16:16 + S], in0=W[src][:, 16:16 + S],
                                                                   in1=W[src][:, 16 - sh:16 - sh + S], op=ALU.add),
                                  reads=[W_b[src]], writes=[W_b[dst]])
                        lvl(1, 0, 1)
                        lvl(2, 1, 2)
                        if c == 0:
                            lo, hi, pt_ = 1, 2, 3
                        else:
                            lvl(3, 2, 4)
                            lvl(1, 3, 8)
                            lo, hi, pt_ = 3, 1, 2
                        for (sel, p0) in ((lo, 0), (hi, 64)):
                            tr.op("dve", [lambda e, sel=sel, p0=p0: e.tensor_scalar(
                                out=W[pt_][p0:p0 + 64, 16:16 + S], in0=W[sel][p0:p0 + 64, 16:16 + S],
                                scalar1=invw[p0:p0 + 64, c:c + 1], scalar2=None, op0=ALU.mult)],
                                reads=[W_b[sel], cst], writes=[W_b[pt_]])
                        for (sel, p0) in ((lo, 0), (hi, 64)):
                            tr.op("dve", [lambda e, sel=sel, p0=p0: e.tensor_tensor(
                                out=W[pt_][p0:p0 + 64, 16:32], in0=W[sel][p0:p0 + 64, 16:32],
                                in1=ic16[p0:p0 + 64, c * 16:(c + 1) * 16], op=ALU.mult)],
                                reads=[W_b[sel], cst], writes=[W_b[pt_]])
                        tr.op("dve", lambda e: e.tensor_tensor(out=pbf[:, :], in0=W[pt_][:, 16:16 + S],
                                                               in1=W[0][:, 16:16 + S], op=ALU.subtract),
                              reads=[W_b[pt_], W_b[0]], writes=[pbf_b])
                        for tg in range(4):
                            bt, bb = nb()
                            tr.op("pe", lambda e, bt=bt, tg=tg: e.matmul(bt[:, :], lhsT=pwbd[:, c, :],
                                                                         rhs=pbf[:, tg * 512:(tg + 1) * 512],
                                                                         start=True, stop=True),
                                  reads=[pwbd_b, pbf_b], writes=[bb])
                            tr.op("act", lambda e, bt=bt, tg=tg: e.activation(
                                out=yT[:, 3 + c, tg * 512:(tg + 1) * 512], in_=bt[:, :], func=AF.Identity,
                                scale=psc[:, l * 2 + c:l * 2 + c + 1]), reads=[bb, cst], writes=[yT_b[3 + c]])

                    tr.dma("pool", wv[:], w_in_l[:, :, 1792:2176], writes=[wv_b])
                    bcast_load(SG, bSG, sgu_ln_g[l:l + 1, :])
                    bcast_load(SBt, bSB, sgu_ln_b[l:l + 1, :])
                    tr.dma("pool", wsr[:], sgu_wT[l].rearrange("h s t -> s h t"), writes=[wsr_b])
                    tr.dma("pool", bsr[:], sgu_b[l:l + 1, :], writes=[bsr_b])
                    for h in range(6):
                        tr.op("dve", lambda e, h=h: e.tensor_tensor(out=wsm[:, h, :], in0=wsr[:, h, :], in1=cmask[:],
                                                                    op=ALU.mult), reads=[wsr_b, cst], writes=[wsm_b])
                    vbank = {}
                    vrs = {}

                    def v_s0(tt):
                        bt, bb = nb()
                        em = [lambda e, k=k: e.matmul(bt[:, 0:384], lhsT=hT[:, k, tt * 128:(tt + 1) * 128],
                                                      rhs=wv[:, k, :], start=(k == 0), stop=(k == 7))
                              for k in range(8)]
                        tr.op("pe", em, reads=[wv_b, hT_b[tt]], writes=[bb])
                        ln_stats(lnt[tt % 4], bt[:, 0:384], bb, 384, 1)
                        vbank[tt] = (bt, bb)

                    def v_s1(tt):
                        vrs[tt] = ln_rstd(lnt[tt % 4])

                    def v_s2(tt):
                        i = tt % 2
                        bt, bb = vbank.pop(tt)
                        rs, nmr, sb_ = vrs.pop(tt)
                        tr.op("act", lambda e: e.activation(
                            out=vt[i][:], in_=bt[:, 0:384], func=AF.Identity, scale=rs[:, 0:1], bias=nmr[:, 0:1]),
                            reads=[bb] + sb_, writes=[vt_b[i]])
                        tr.op("dve", lambda e: e.tensor_tensor(out=vt2[i][:], in0=vt[i][:], in1=SG[:], op=ALU.mult),
                              reads=[vt_b[i], bSG], writes=[vt2_b[i]])
                        tr.op("dve", lambda e: e.tensor_tensor(out=vnb[:, tt, :], in0=vt2[i][:], in1=SBt[:], op=ALU.add),
                              reads=[vt2_b[i], bSB], writes=[vnb_b[tt]])
                    pipeline(NT, [v_s0, v_s1, v_s2], "SV")
                    for pr in range(3):
                        bk = zmm(11 + pr)
                        for tg in range(4):
                            tr.op("act", lambda e, tg=tg, bt=bk[tg][0]: e.activation(
                                out=W[0][:, tg * 512:(tg + 1) * 512], in_=bt[:, :], func=AF.Identity),
                                reads=[bk[tg][1]], writes=[W_b[0]])
                        for hh in range(2):
                            h = 2 * pr + hh
                            p0 = hh * 64
                            for tg in range(4):
                                bt, bb = nb()
                                em = []
                                for q in range(4):
                                    tt = tg * 4 + q
                                    em.append(lambda e, bt=bt, q=q, tt=tt: e.matmul(
                                        bt[:, q * 128:(q + 1) * 128], lhsT=vnb[:, tt, pr * 128:(pr + 1) * 128],
                                        rhs=wsm[:, h, :], start=True, stop=False))
                                    em.append(lambda e, bt=bt, q=q: e.matmul(
                                        bt[:, q * 128:(q + 1) * 128], lhsT=ones_b[0:1, :],
                                        rhs=bsr[0:1, h * 128:(h + 1) * 128], start=False, stop=True))
                                tr.op("pe", em, reads=[wsm_b, bsr_b, cst] + vnb_b[tg * 4:(tg + 1) * 4], writes=[bb])
                                tr.op("dve", lambda e, bt=bt, tg=tg: e.tensor_tensor(
                                    out=yT[p0:p0 + 64, 5 + pr, tg * 512:(tg + 1) * 512],
                                    in0=W[0][p0:p0 + 64, tg * 512:(tg + 1) * 512], in1=bt[p0:p0 + 64, :], op=ALU.mult),
                                    reads=[W_b[0], bb], writes=[yT_b[5 + pr]])
                    tr.barrier()

                with ExitStack() as p3:
                    wo = sb(p3, "wo", [128, 8, D], BF16); wo_b = Buf("wo")
                    tr.dma("pool", wo[:], w_out[l].rearrange("(k p) n -> p k n", p=128), writes=[wo_b])
                    for k in range(8):
                        tr.op("dve" if k % 2 == 0 else "pool", lambda e, k=k: e.tensor_tensor(
                            out=wo[:, k, :], in0=wo[:, k, :], in1=G1[:], op=ALU.mult), reads=[wo_b, bG1], writes=[wo_b])
                    GM = sb(p3, "GM", [128, D]); BM = sb(p3, "BM", [128, D]); bGM = Buf("GM"); bBM = Buf("BM")
                    tr.op("dve", lambda e: e.tensor_tensor(out=GM[:], in0=LG[:], in1=M2[:], op=ALU.mult),
                          reads=[bLG, bM2], writes=[bGM])
                    tr.op("dve", lambda e: e.tensor_tensor(out=BM[:], in0=LB[:], in1=M2[:], op=ALU.mult),
                          reads=[bLB, bM2], writes=[bBM])
                    tr.op("pool", lambda e: e.tensor_tensor(out=BM[:], in0=BM[:], in1=S2[:], op=ALU.add),
                          reads=[bBM, bS2], writes=[bBM])
                    uu = [sb(p3, "uu%d" % i, [128, D]) for i in range(4)]; uu_b = [Buf("uu%d" % i) for i in range(4)]
                    xn = [sb(p3, "xn%d" % i, [128, D]) for i in range(2)]; xn_b = [Buf("xna"), Buf("xnb")]
                    x1 = [sb(p3, "x1%d" % i, [128, D]) for i in range(2)]; x1_b = [Buf("x1a"), Buf("x1b")]
                    tb = [sb(p3, "tb%d" % i, [128, D]) for i in range(2)]; tb_b = [Buf("tba"), Buf("tbb")]
                    lnt = ln_bufs(p3, 4)

                    def a3_ld(tt):
                        tr.dma("sp", xt[tt % 3][:], x_src[tt * 128:(tt + 1) * 128, :], writes=[xt_b[tt % 3]])

                    def a3_sA(tt):
                        i = tt % 2
                        i4 = tt % 4
                        for half in range(2):
                            bt, bb = nb()
                            em = [lambda e, k=k, bt=bt, half=half: e.matmul(
                                bt[:, :], lhsT=yT[:, k, tt * 128:(tt + 1) * 128], rhs=wo[:, k, half * 512:(half + 1) * 512],
                                start=(k == 0), stop=(k == 7)) for k in range(8)]
                            tr.op("pe", em, reads=[wo_b] + yT_b, writes=[bb])
                            tr.op("dve", lambda e, bt=bt, half=half: e.scalar_tensor_tensor(
                                out=uu[i4][:, half * 512:(half + 1) * 512], in0=xt[tt % 3][:, half * 512:(half + 1) * 512],
                                scalar=ALPHA, in1=bt[:, :], op0=ALU.mult, op1=ALU.add),
                                reads=[xt_b[tt % 3], bb], writes=[uu_b[i4]])
                        ln_stats(lnt[i4], uu[i4], uu_b[i4], D, 2)

                    rsn = {}
                    tbank = {}

                    def a3_s1(tt):
                        ln_sqrt(lnt[tt % 4])

                    def a3_s2(tt):
                        rsn[tt] = ln_rs(lnt[tt % 4])

                    def a3_s3(tt):
                        i = tt % 2
                        i4 = tt % 4
                        rs, nmr, sb_ = rsn.pop(tt)
                        tr.op("act", lambda e: e.activation(out=xn[i][:], in_=uu[i4][:], func=AF.Identity,
                                                            scale=rs[:, 0:1], bias=nmr[:, 0:1]),
                              reads=[uu_b[i4]] + sb_, writes=[xn_b[i]])

                    def a3_s4(tt):
                        i = tt % 2
                        tr.op("dve", lambda e: e.tensor_tensor(out=tmp[i][:], in0=xn[i][:], in1=LG[:], op=ALU.mult),
                              reads=[xn_b[i], bLG], writes=[tmp_b[i]])
                        tr.op("dve", lambda e: e.tensor_tensor(out=x1[i][:], in0=tmp[i][:], in1=LB[:], op=ALU.add),
                              reads=[tmp_b[i], bLB], writes=[x1_b[i]])
                        tr.dma("pool", xs[tt * 128:(tt + 1) * 128, :], x1[i][:], reads=[x1_b[i]])
                        tr.op("dve", lambda e: e.tensor_tensor(out=tb[i][:], in0=xn[i][:], in1=GM[:], op=ALU.mult),
                              reads=[xn_b[i], bGM], writes=[tb_b[i]])
                        tr.op("pool", lambda e: e.tensor_tensor(out=hb[i][:], in0=tb[i][:], in1=BM[:], op=ALU.add),
                              reads=[tb_b[i], bBM], writes=[hb_b[i]])
                        tr.dma("pool", h2d[tt * 128:(tt + 1) * 128, :], hb[i][:], reads=[hb_b[i]], writes=[h2d_b[tt]])

                    def a3_s5(tt):
                        i = tt % 2
                        bt, bb = nb()
                        btb = bt.bitcast(BF16)
                        em = [lambda e, k=k: e.transpose(btb[:, k * 128:(k + 1) * 128], hb[i][:, k * 128:(k + 1) * 128],
                                                         ident[:]) for k in range(8)]
                        tr.op("pe", em, reads=[hb_b[i], cst], writes=[bb])
                        tbank[tt] = (btb, bb)

                    def a3_s6(tt):
                        btb, bb = tbank.pop(tt)
                        tr.op("act", lambda e: e.activation(out=hT[:, :, tt * 128:(tt + 1) * 128],
                                                            in_=btb[:, 0:1024].rearrange("p (k t) -> p k t", k=8),
                                                            func=AF.Identity), reads=[bb], writes=[hT_b[tt]])
                    pipeline(NT, [a3_ld, a3_sA, a3_s1, a3_s2, a3_s3, a3_s4, a3_s5, a3_s6], "A3")
                    tr.barrier()

            with ExitStack() as pb:
                G2 = sb(pb, "G2", [128, D]); LG = sb(pb, "LG2", [128, D]); LB = sb(pb, "LB2", [128, D])
                bG2, bLG, bLB = Buf("G2"), Buf("LG2"), Buf("LB2")
                with ExitStack() as pt:
                    wa = [sb(pt, "wa%d" % i, [128, 8, 512]) for i in range(2)]
                    wa_b = [Buf("wa0"), Buf("wa1")]
                    brow = sb(pt, "brow", [1, 6 * D]); brow_b = Buf("brow")
                    tr.dma("sp", brow[:], b_ada[l:l + 1, :], writes=[brow_b])
                    wab = [sb(pt, "wab%d" % i, [128, 8, 512], BF16) for i in range(2)]
                    wab_b = [Buf("wab0"), Buf("wab1")]
                    ada_chunk((wa, wa_b, brow, brow_b, wab, wab_b), l, 5, G2, bG2, False)
                    bcast_load(LG, bLG, ln2_g[l:l + 1, :])
                    bcast_load(LB, bLB, ln2_b[l:l + 1, :])
                    tr.barrier()
                idx_hi = sb(pb, "idx_hi", [128, 16], I32); idx_lo = sb(pb, "idx_lo", [128, 16], I32)
                whi = sb(pb, "whi", [128, 16]); wlo = sb(pb, "wlo", [128, 16])
                idx_b = Buf("idx"); wgt_b = Buf("wgt")
                yg_b = [Buf("yg%d" % i) for i in range(NE)]
                with ExitStack() as pm:
                    wg = [sb(pm, "wg%d" % i, [128, 8, DFF], BF16) for i in range(2)]
                    wu = [sb(pm, "wu%d" % i, [128, 8, DFF], BF16) for i in range(2)]
                    wd = [sb(pm, "wd%d" % i, [128, 4, D], BF16) for i in range(2)]
                    wg_b = [Buf("wg0"), Buf("wg1")]; wu_b = [Buf("wu0"), Buf("wu1")]; wd_b = [Buf("wd0"), Buf("wd1")]
                    sl = [sb(pm, "sl%d" % i, [128, 512]) for i in range(2)]; sl_b = [Buf("sla"), Buf("slb")]
                    RL = sb(pm, "RL", [128, 512]); RE = sb(pm, "RE", [128, 512]); RT = sb(pm, "RT", [128, 512])
                    RM = sb(pm, "RM", [128, 512]); Wt = sb(pm, "Wt", [128, 512])
                    r16 = sb(pm, "r16", [128, 16]); g16 = sb(pm, "g16", [128, 16]); rg16 = sb(pm, "rg16", [128, 16])
                    m1 = sb(pm, "m1", [128, 64]); m2 = sb(pm, "m2", [128, 64]); scg = sb(pm, "scg", [128, 64])
                    gs = sb(pm, "gs", [128, 64])
                    rb = Buf("route")
                    Wt_b = Buf("Wt")

                    def wprefetch(ex):
                        i = ex % 2
                        tr.dma("pool", wg[i][:], w_gate[l, ex].rearrange("(p k) n -> p k n", p=128), writes=[wg_b[i]])
                        tr.dma("pool", wu[i][:], w_up[l, ex].rearrange("(p k) n -> p k n", p=128), writes=[wu_b[i]])
                        tr.dma("pool", wd[i][:], w_down[l, ex].rearrange("(p k) n -> p k n", p=128), writes=[wd_b[i]])
                    wprefetch(0)
                    wprefetch(1)
                    hsA = sb(pm, "hsA", [128, NT, D], BF16); hsA_b = [Buf("hsA%d" % i) for i in range(NT)]
                    for tt in range(NT):
                        tr.dma("sp", hsA[:, tt, :], h2d[tt * 128:(tt + 1) * 128, :], reads=[h2d_b[tt]], writes=[hsA_b[tt]])

                    bt, bb = nb()
                    em = []
                    for tt in range(NT):
                        for k in range(8):
                            em.append(lambda e, k=k, tt=tt: e.matmul(bt[:, tt * 32:(tt + 1) * 32],
                                                                     lhsT=hT[:, k, tt * 128:(tt + 1) * 128], rhs=wr[:, k, :],
                                                                     start=(k == 0), stop=False))
                        em.append(lambda e, tt=tt: e.matmul(bt[:, tt * 32:(tt + 1) * 32], lhsT=ones_b[0:1, :], rhs=brr[0:1, :],
                                                            start=False, stop=True))
                    tr.op("pe", em, reads=hT_b + [cst], writes=[bb])
                    tr.op("act", lambda e: e.activation(out=RL[:], in_=bt[:, :], func=AF.Identity), reads=[bb], writes=[rb])

                    def v3(t, a, b):
                        return t[:, :].rearrange("p (a b) -> p a b", a=a)

                    def bc3(t, a, b):
                        return t[:, :].unsqueeze(2).to_broadcast([128, a, b])

                    def rop(f):
                        tr.op("dve", f, reads=[rb], writes=[rb])
                    rop(lambda e: e.tensor_reduce(out=r16[:, :], in_=v3(RL, 16, 32), axis=AX.X, op=ALU.max))
                    rop(lambda e: e.tensor_tensor(out=v3(RT, 16, 32), in0=v3(RL, 16, 32), in1=bc3(r16, 16, 32), op=ALU.subtract))
                    tr.op("act", lambda e: e.activation(out=RE[:], in_=RT[:], func=AF.Exp), reads=[rb], writes=[rb])
                    rop(lambda e: e.tensor_reduce(out=m1[:, :], in_=v3(RE, 64, 8), axis=AX.X, op=ALU.max))
                    rop(lambda e: e.tensor_tensor(out=v3(RT, 64, 8), in0=v3(RE, 64, 8), in1=bc3(m1, 64, 8), op=ALU.is_equal))
                    rop(lambda e: e.tensor_tensor(out=RT[:], in0=RT[:], in1=RE[:], op=ALU.mult))
                    rop(lambda e: e.tensor_tensor(out=RM[:], in0=RE[:], in1=RT[:], op=ALU.subtract))
                    rop(lambda e: e.tensor_reduce(out=m2[:, :], in_=v3(RM, 64, 8), axis=AX.X, op=ALU.max))
                    rop(lambda e: e.tensor_tensor(out=scg[:], in0=m1[:], in1=m2[:], op=ALU.add))
                    rop(lambda e: e.tensor_reduce(out=g16[:, :], in_=v3(scg, 16, 4), axis=AX.X, op=ALU.max))
                    rop(lambda e: e.tensor_tensor(out=v3(gs, 16, 4), in0=v3(scg, 16, 4), in1=bc3(g16, 16, 4), op=ALU.is_equal))
                    rop(lambda e: e.tensor_tensor(out=v3(RT, 64, 8), in0=v3(RE, 64, 8), in1=bc3(m2, 64, 8), op=ALU.is_ge))
                    rop(lambda e: e.tensor_tensor(out=v3(RM, 64, 8), in0=v3(RT, 64, 8), in1=bc3(gs, 64, 8), op=ALU.mult))
                    rop(lambda e: e.reciprocal(out=rg16[:], in_=g16[:]))
                    rop(lambda e: e.tensor_tensor(out=RT[:], in0=RE[:], in1=RM[:], op=ALU.mult))
                    tr.op("dve", lambda e: e.tensor_tensor(out=v3(Wt, 16, 32), in0=v3(RT, 16, 32), in1=bc3(rg16, 16, 32),
                                                           op=ALU.mult), reads=[rb], writes=[Wt_b])

                    maskb = sb(pm, "maskb", [128, 512], BF16)
                    cntf = sb(pm, "cntf", [128, NE]); nbf = sb(pm, "nbf", [128, NE]); nbi = sb(pm, "nbi", [128, NE], I32)
                    f16a = sb(pm, "f16a", [128, 16]); f16b = sb(pm, "f16b", [128, 16])
                    rop(lambda e: e.tensor_copy(out=maskb[:], in_=RM[:]))
                    r2, r2b = nb()
                    em = []
                    for tt in range(NT):
                        for j in range(tt):
                            em.append(lambda e, tt=tt, j=j: e.matmul(r2[:, tt * 32:(tt + 1) * 32], lhsT=ones128[:],
                                                                     rhs=maskb[:, j * 32:(j + 1) * 32],
                                                                     start=(j == 0), stop=False))
                        em.append(lambda e, tt=tt: e.matmul(r2[:, tt * 32:(tt + 1) * 32], lhsT=ltri[:],
                                                            rhs=maskb[:, tt * 32:(tt + 1) * 32],
                                                            start=(tt == 0), stop=True))
                    tr.op("pe", em, reads=[rb, cst], writes=[r2b])
                    r3, r3b = nb()
                    em = [lambda e, j=j: e.matmul(r3[:, 0:32], lhsT=ones128[:], rhs=maskb[:, j * 32:(j + 1) * 32],
                                                  start=(j == 0), stop=(j == NT - 1)) for j in range(NT)]
                    tr.op("pe", em, reads=[rb, cst], writes=[r3b])
                    tr.op("dve", lambda e: e.tensor_tensor(out=RT[:], in0=r2[:, :], in1=ecol[:], op=ALU.add),
                          reads=[r2b, cst, rb], writes=[rb])
                    tr.op("dve", lambda e: e.tensor_scalar(out=RL[:], in0=r2[:, :], scalar1=float(CAP), scalar2=None,
                                                           op0=ALU.is_lt), reads=[r2b, rb], writes=[rb])
                    rop(lambda e: e.tensor_tensor(out=RE[:], in0=RM[:], in1=RL[:], op=ALU.mult))
                    rop(lambda e: e.tensor_tensor(out=RT[:], in0=RT[:], in1=RE[:], op=ALU.mult))
                    tr.op("dve", lambda e: e.tensor_tensor(out=Wt[:], in0=Wt[:], in1=RL[:], op=ALU.mult),
                          reads=[rb, Wt_b], writes=[Wt_b])
                    rop(lambda e: e.tensor_reduce(out=r16[:, :], in_=v3(RT, 16, 32), axis=AX.X, op=ALU.max))
                    rop(lambda e: e.tensor_reduce(out=g16[:, :], in_=v3(RT, 16, 32), axis=AX.X, op=ALU.add))
                    f16c = sb(pm, "f16c", [128, 16])
                    rop(lambda e: e.tensor_scalar(out=f16c[:], in0=r16[:], scalar1=0.5, scalar2=None, op0=ALU.is_lt))
                    rop(lambda e: e.tensor_scalar(out=f16a[:], in0=r16[:], scalar1=-1.0, scalar2=None, op0=ALU.add))
                    rop(lambda e: e.scalar_tensor_tensor(out=f16a[:], in0=f16c[:], scalar=float(NROWS + 1), in1=f16a[:],
                                                         op0=ALU.mult, op1=ALU.add))
                    rop(lambda e: e.tensor_tensor(out=f16b[:], in0=g16[:], in1=r16[:], op=ALU.subtract))
                    rop(lambda e: e.tensor_scalar(out=f16c[:], in0=f16b[:], scalar1=0.5, scalar2=None, op0=ALU.is_lt))
                    rop(lambda e: e.tensor_scalar(out=f16b[:], in0=f16b[:], scalar1=-1.0, scalar2=None, op0=ALU.add))
                    rop(lambda e: e.scalar_tensor_tensor(out=f16b[:], in0=f16c[:], scalar=float(NROWS + 1), in1=f16b[:],
                                                         op0=ALU.mult, op1=ALU.add))
                    tr.op("dve", lambda e: e.tensor_copy(out=idx_hi[:], in_=f16a[:]), reads=[rb], writes=[idx_b])
                    tr.op("dve", lambda e: e.tensor_copy(out=idx_lo[:], in_=f16b[:]), reads=[rb], writes=[idx_b])
                    rop(lambda e: e.tensor_tensor(out=v3(RL, 16, 32), in0=v3(RT, 16, 32), in1=bc3(r16, 16, 32),
                                                  op=ALU.is_equal))
                    tr.op("dve", lambda e: e.tensor_tensor(out=RL[:], in0=RL[:], in1=Wt[:], op=ALU.mult),
                          reads=[rb, Wt_b], writes=[rb])
                    tr.op("dve", lambda e: e.tensor_reduce(out=whi[:, :], in_=v3(RL, 16, 32), axis=AX.X, op=ALU.add),
                          reads=[rb], writes=[wgt_b])
                    tr.op("dve", lambda e: e.tensor_reduce(out=f16a[:, :], in_=v3(Wt, 16, 32), axis=AX.X, op=ALU.add),
                          reads=[Wt_b, rb], writes=[rb])
                    tr.op("dve", lambda e: e.tensor_tensor(out=wlo[:], in0=f16a[:], in1=whi[:], op=ALU.subtract),
                          reads=[rb, wgt_b], writes=[wgt_b])
                    tr.op("dve", lambda e: e.tensor_copy(out=cntf[:], in_=r3[:, 0:32]), reads=[r3b, rb], writes=[rb])
                    rop(lambda e: e.tensor_scalar(out=nbf[:], in0=cntf[:], scalar1=0.0, scalar2=None, op0=ALU.is_gt))
                    for j in range(1, JB):
                        rop(lambda e, j=j: e.scalar_tensor_tensor(out=nbf[:], in0=cntf[:], scalar=float(128 * j),
                                                                  in1=nbf[:], op0=ALU.is_gt, op1=ALU.add))
                    rop(lambda e: e.tensor_copy(out=nbi[:], in_=nbf[:]))
                    nblk_b = Buf("nblk")
                    tr.dma("sp", nblk_d, nbi[0:1, :], reads=[rb], writes=[nblk_b])

                    xgw = [Buf("xgw%d" % i) for i in range(2 * NT)]
                    for tt in range(NT):
                        tr.idma(xg[:, :], idx_hi[:, tt:tt + 1], hsA[:, tt, :], None, None,
                                reads=[hsA_b[tt], idx_b], writes=[xgw[2 * tt]])
                        tr.idma(xg[:, :], idx_lo[:, tt:tt + 1], hsA[:, tt, :], None, None,
                                reads=[hsA_b[tt], idx_b], writes=[xgw[2 * tt + 1]])

                    xb = [sb(pm, "xb%d" % i, [128, D], BF16) for i in range(2)]; xb_b = [Buf("xb0"), Buf("xb1")]
                    xT = [sb(pm, "xT%d" % i, [128, 8, 128], BF16) for i in range(2)]; xT_b = [Buf("xT0"), Buf("xT1")]
                    ab = [sb(pm, "ab%d" % i, [128, DFF], BF16) for i in range(2)]; ab_b = [Buf("ab0"), Buf("ab1")]
                    aT = [sb(pm, "aT%d" % i, [128, 4, 128], BF16) for i in range(2)]; aT_b = [Buf("aT0"), Buf("aT1")]
                    yb = [sb(pm, "yb%d" % i, [128, D]) for i in range(2)]; yb_b = [Buf("yb0"), Buf("yb1")]
                    blocks = [(ex, j) for ex in range(NE) for j in range(JB)]

                    def prologue(ex):
                        if ex >= 2:
                            wprefetch(ex)
                        for en in ("pe", "act", "dve", "pool", "sp"):
                            tr.wait(en, nblk_b.w)
                        for r in cregs[ex % 3]:
                            nc.reg_load(r, nblk_d[0:1, ex:ex + 1])

                    def e_s1(b):
                        ex, j = blocks[b]
                        c = b % 2
                        if j == 0:
                            prologue(ex)

                        def body():
                            r0 = ex * CAP + j * 128
                            tr.dma("sp", xb[c][:], xg[r0:r0 + 128, :], reads=xgw, writes=[xb_b[c]])
                            bt, bb = nb()
                            btb = bt.bitcast(BF16)
                            em = [lambda e, k=k: e.transpose(btb[:, k * 128:(k + 1) * 128], xb[c][:, k:1024:8],
                                                             ident[:]) for k in range(8)]
                            tr.op("pe", em, reads=[xb_b[c], cst], writes=[bb])
                            tr.op("act", lambda e: e.activation(out=xT[c][:, :, :],
                                                                in_=btb[:, 0:1024].rearrange("p (k t) -> p k t", k=8),
                                                                func=AF.Identity), reads=[bb], writes=[xT_b[c]])
                        tr.cond_block(cregs[ex % 3], j, body)

                    def e_s2(b):
                        ex, j = blocks[b]
                        c = b % 2
                        i = ex % 2

                        def body():
                            bg, bgb = nb()
                            em = [lambda e, k=k: e.matmul(bg[:, :], lhsT=xT[c][:, k, :], rhs=wg[i][:, k, :],
                                                          start=(k == 0), stop=(k == 7)) for k in range(8)]
                            tr.op("pe", em, reads=[xT_b[c], wg_b[i]], writes=[bgb])
                            bu, bub = nb()
                            em = [lambda e, k=k: e.matmul(bu[:, :], lhsT=xT[c][:, k, :], rhs=wu[i][:, k, :],
                                                          start=(k == 0), stop=(k == 7)) for k in range(8)]
                            tr.op("pe", em, reads=[xT_b[c], wu_b[i]], writes=[bub])
                            tr.op("act", lambda e: e.activation(out=sl[c][:], in_=bg[:, :], func=AF.Silu),
                                  reads=[bgb], writes=[sl_b[c]])
                            tr.op("dve", lambda e: e.tensor_tensor(out=ab[c][:], in0=sl[c][:], in1=bu[:, :], op=ALU.mult),
                                  reads=[sl_b[c], bub], writes=[ab_b[c]])
                        tr.cond_block(cregs[ex % 3], j, body)

                    def e_s3(b):
                        ex, j = blocks[b]
                        c = b % 2

                        def body():
                            b2, b2b = nb()
                            b2v = b2.bitcast(BF16)
                            em = [lambda e, k=k: e.transpose(b2v[:, k * 128:(k + 1) * 128], ab[c][:, k:512:4],
                                                             ident[:]) for k in range(4)]
                            tr.op("pe", em, reads=[ab_b[c], cst], writes=[b2b])
                            tr.op("act", lambda e: e.activation(out=aT[c][:, :, :],
                                                                in_=b2v[:, 0:512].rearrange("p (k t) -> p k t", k=4),
                                                                func=AF.Identity), reads=[b2b], writes=[aT_b[c]])
                        tr.cond_block(cregs[ex % 3], j, body)

                    def e_s4(b):
                        ex, j = blocks[b]
                        c = b % 2
                        i = ex % 2

                        def body():
                            r0 = ex * CAP + j * 128
                            for half in range(2):
                                bo, bob = nb()
                                em = [lambda e, k=k, bo=bo: e.matmul(bo[:, :], lhsT=aT[c][:, k, :],
                                                                     rhs=wd[i][:, k, half * 512:(half + 1) * 512],
                                                                     start=(k == 0), stop=(k == 3)) for k in range(4)]
                                tr.op("pe", em, reads=[aT_b[c], wd_b[i]], writes=[bob])
                                if half == 0:
                                    tr.op("act", lambda e, bo=bo: e.activation(out=yb[c][:, 0:512], in_=bo[:, :],
                                                                               func=AF.Identity),
                                          reads=[bob], writes=[yb_b[c]])
                                else:
                                    tr.op("dve", lambda e, bo=bo: e.tensor_copy(out=yb[c][:, 512:1024], in_=bo[:, :]),
                                          reads=[bob], writes=[yb_b[c]])
                            tr.dma("sp", yg[r0:r0 + 128, :], yb[c][:], reads=[yb_b[c]], writes=[yg_b[ex]])
                        tr.cond_block(cregs[ex % 3], j, body)
                    pipeline(len(blocks), [e_s1, e_s2, e_s3, e_s4], "EX")
                    tr.barrier()

                with ExitStack() as p4:
                    def mk(nm, n):
                        return [sb(p4, "%s%d" % (nm, i), [128, D]) for i in range(n)], [Buf("%s%d" % (nm, i)) for i in range(n)]
                    xt, xt_b = mk("xt", 4); yh, yh_b = mk("yh", 2); yl, yl_b = mk("yl", 3)
                    t1, t1_b = mk("t1", 2); ua, ua_b = mk("ua", 2); t2, t2_b = mk("t2", 2)
                    uu, uu_b = mk("uu", 4); xn, xn_b = mk("xn", 2); ub, ub_b = mk("ub", 2); xo, xo_b = mk("xo", 2)
                    lnt = ln_bufs(p4, 4)
                    rsn = {}

                    def l_s0(tt):
                        tr.dma("sp", xt[tt % 4][:], xs[tt * 128:(tt + 1) * 128, :], writes=[xt_b[tt % 4]])
                        tr.idma(yh[tt % 2][:], None, yg[:, :], idx_hi[:, tt:tt + 1], None,
                                reads=yg_b + [idx_b], writes=[yh_b[tt % 2]])
                        tr.idma(yl[tt % 3][:], None, yg[:, :], idx_lo[:, tt:tt + 1], None,
                                reads=yg_b + [idx_b], writes=[yl_b[tt % 3]])

                    def l_s1(tt):
                        i = tt % 2
                        tr.op("act", lambda e: e.activation(out=t1[i][:], in_=yh[i][:], func=AF.Identity,
                                                            scale=whi[:, tt:tt + 1]),
                              reads=[yh_b[i], wgt_b], writes=[t1_b[i]])

                    def l_s2(tt):
                        i = tt % 2
                        tr.op("dve", lambda e: e.scalar_tensor_tensor(out=ua[i][:], in0=yl[tt % 3][:],
                                                                      scalar=wlo[:, tt:tt + 1], in1=t1[i][:],
                                                                      op0=ALU.mult, op1=ALU.add),
                              reads=[yl_b[tt % 3], t1_b[i], wgt_b], writes=[ua_b[i]])
                        tr.op("pool" if tt % 3 == 2 else "dve",
                              lambda e: e.tensor_tensor(out=t2[i][:], in0=ua[i][:], in1=G2[:], op=ALU.mult),
                              reads=[ua_b[i], bG2], writes=[t2_b[i]])

                    def l_s3(tt):
                        i = tt % 2
                        i4 = tt % 4
                        tr.op("dve", lambda e: e.scalar_tensor_tensor(out=uu[i4][:], in0=xt[i4][:], scalar=ALPHA,
                                                                      in1=t2[i][:], op0=ALU.mult, op1=ALU.add),
                              reads=[xt_b[i4], t2_b[i]], writes=[uu_b[i4]])
                        ln_stats(lnt[i4], uu[i4], uu_b[i4], D, 2)

                    def l_s4(tt):
                        ln_sqrt(lnt[tt % 4])

                    def l_s5(tt):
                        rsn[tt] = ln_rs(lnt[tt % 4])

                    def l_s6(tt):
                        i = tt % 2
                        i4 = tt % 4
                        rs, nmr, sb_ = rsn.pop(tt)
                        tr.op("act", lambda e: e.activation(out=xn[i][:], in_=uu[i4][:], func=AF.Identity,
                                                            scale=rs[:, 0:1], bias=nmr[:, 0:1]),
                              reads=[uu_b[i4]] + sb_, writes=[xn_b[i]])

                    def l_s7(tt):
                        i = tt % 2
                        tr.op("dve", lambda e: e.tensor_tensor(out=ub[i][:], in0=xn[i][:], in1=LG[:], op=ALU.mult),
                              reads=[xn_b[i], bLG], writes=[ub_b[i]])
                        tr.op("pool" if tt % 3 == 0 else "dve",
                              lambda e: e.tensor_tensor(out=xo[i][:], in0=ub[i][:], in1=LB[:], op=ALU.add),
                              reads=[ub_b[i], bLB], writes=[xo_b[i]])
                        tr.dma("pool", x_dst[tt * 128:(tt + 1) * 128, :], xo[i][:], reads=[xo_b[i]])
                    pipeline(NT, [l_s0, l_s1, l_s2, l_s3, l_s4, l_s5, l_s6, l_s7], "LN")
                    tr.barrier()
            x_src = xs
        tr.barrier(engines=["sp"])
    return nc


def _prep_inputs(inp):
    f = np.float32
    g = lambda k: np.ascontiguousarray(np.asarray(inp[k], dtype=f))
    conv_w = g("conv_w"); conv_b = g("conv_b")
    cw = np.zeros((128, DEPTH, 3, 4), f)
    for l in range(DEPTH):
        for j in range(3):
            for k in range(3):
                cw[:, l, j, k] = conv_w[l, k, j * 128:(j + 1) * 128]
            cw[:, l, j, 3] = conv_b[l, j * 128:(j + 1) * 128]
    psc = np.ascontiguousarray(g("pool_scale").reshape(DEPTH, 2, 128).transpose(2, 0, 1).reshape(128, DEPTH * 2))
    t = np.arange(128)
    cmask = (t[:, None] <= t[None, :]).astype(f)
    wins = (2, 4, 8, 16)
    invw = np.zeros((128, 2), f); ic16 = np.zeros((128, 2, 16), f)
    for c in range(2):
        for hh in range(2):
            w = wins[c * 2 + hh]
            invw[hh * 64:(hh + 1) * 64, c] = 1.0 / w
            ic16[hh * 64:(hh + 1) * 64, c, :] = 1.0 / np.minimum(np.arange(16) + 1, w)
    shared = {
        "w_ada": g("w_ada"), "b_ada": g("b_ada"), "w_in": g("w_in"),
        "cw": cw.reshape(128, -1), "pool_w": g("pool_w"), "psc": psc,
        "sgu_ln_g": g("sgu_ln_g"), "sgu_ln_b": g("sgu_ln_b"),
        "sgu_wT": np.ascontiguousarray(g("sgu_w").transpose(0, 1, 3, 2)),
        "sgu_b": g("sgu_b").reshape(DEPTH, 6 * 128), "w_out": g("w_out"),
        "ln1_g": g("ln1_g"), "ln1_b": g("ln1_b"), "w_router": g("w_router"),
        "b_router": g("b_router").reshape(1, NE), "w_gate": g("w_gate"), "w_up": g("w_up"),
        "w_down": g("w_down"), "ln2_g": g("ln2_g"), "ln2_b": g("ln2_b"),
        "ident": np.eye(128, dtype=f), "cmask": cmask, "invw": invw, "ic16": ic16.reshape(128, 32),
        "ltri": (t[:, None] < t[None, :]).astype(f),
        "ecol": np.ascontiguousarray(np.broadcast_to(np.tile(np.arange(NE, dtype=f) * CAP + 1.0, NT)[None, :], (128, NT * NE))),
    }
    x = g("x"); c = g("c")
    maps = []
    for b in range(8):
        m = dict(shared)
        m["x"] = x[b]
        m["cT"] = np.ascontiguousarray(c[b].reshape(8, 128).T)
        maps.append(m)
    return maps


_NC_CACHE = {}


def kernel(**inputs):
    n_layers = DEPTH
    if n_layers not in _NC_CACHE:
        _NC_CACHE[n_layers] = build(n_layers)
    nc = _NC_CACHE[n_layers]
    maps = _prep_inputs(inputs)
    res = run_bass_kernel_spmd(nc, maps, core_ids=list(range(8)))
    return np.stack([np.asarray(r["y"], dtype=np.float32) for r in res.results], axis=0)
```
